# Optimizing a Trainium2 kernel written in Bass

```python
import jax, jax.numpy as jnp
from jax import lax
import numpy as np

D_MODEL = 1024
BATCH = 8
SEQ = 4096
DEPTH = 1

D_MIX = D_MODEL
D_LRU = 512
LRU_BLOCKS = 8
LRU_BLOCK_W = D_LRU // LRU_BLOCKS
LRU_C = 8.0
D_GDN = D_MIX - D_LRU
GDN_HEADS = 4
GDN_HEAD_DIM = D_GDN // GDN_HEADS
CONV_W = 4
CHUNK = 64
N_GROUPS = 4
EXPERTS_PER_GROUP = 8
N_EXPERTS = N_GROUPS * EXPERTS_PER_GROUP
TOP_K = 2
D_EXPERT = 512
MOE_BLOCK = 256
D_PLE = 256
EPS = 1e-6
IN_COLS = 2 * D_LRU + 4 * D_GDN + 2 * GDN_HEADS

kernel_name = 'hymba_rglru_gdn_hmoe_layer'


def rmsnorm(x, g):
    xf = x.astype(jnp.float32)
    return xf * lax.rsqrt(jnp.mean(xf * xf, axis=-1, keepdims=True) + EPS) * g


def l2norm(x):
    return x * lax.rsqrt(jnp.sum(x * x, axis=-1, keepdims=True) + EPS)


def causal_depthwise_conv(x, w):
    s = x.shape[1]
    kw = w.shape[0]
    xp = jnp.pad(x, ((0, 0), (kw - 1, 0), (0, 0)))
    return sum(xp[:, j:j + s] * w[j] for j in range(kw))


def rglru_group(xb, gb, conv_w, conv_b, wa, ba, wi, bi, lam, out_g):
    bsz, s, _ = xb.shape
    xc = causal_depthwise_conv(xb, conv_w) + conv_b
    xblk = xc.reshape(bsz, s, LRU_BLOCKS, LRU_BLOCK_W)
    r = jax.nn.sigmoid(jnp.einsum('bsnc,ncd->bsnd', xblk, wa).reshape(bsz, s, D_LRU) + ba)
    i = jax.nn.sigmoid(jnp.einsum('bsnc,ncd->bsnd', xblk, wi).reshape(bsz, s, D_LRU) + bi)
    log_a = LRU_C * r * jax.nn.log_sigmoid(lam)
    a = jnp.exp(log_a)
    u = jnp.sqrt(-jnp.expm1(2.0 * log_a)) * (i * xc)

    def combine(c1, c2):
        a1, b1 = c1
        a2, b2 = c2
        return a1 * a2, a2 * b1 + b2

    _, h = lax.associative_scan(combine, (a, u), axis=1)
    return rmsnorm(h * jax.nn.gelu(gb), out_g)


def chunked_gated_delta_rule(q, k, v, beta, g):
    bsz, s, nh, dk = q.shape
    dv = v.shape[-1]
    n = s // CHUNK

    def to_chunks(t):
        return t.reshape(bsz, n, CHUNK, nh, -1).transpose(1, 0, 3, 2, 4)

    qc = to_chunks(q) * (dk ** -0.5)
    kc = to_chunks(k)
    vc = to_chunks(v)
    bc = to_chunks(beta[..., None])[..., 0]
    gc = jnp.cumsum(to_chunks(g[..., None])[..., 0], axis=-1)
    causal = jnp.tril(jnp.ones((CHUNK, CHUNK), dtype=bool))
    strict = jnp.tril(jnp.ones((CHUNK, CHUNK), dtype=bool), k=-1)
    diff = gc[..., :, None] - gc[..., None, :]
    decay = jnp.where(causal, jnp.exp(jnp.where(causal, diff, 0.0)), 0.0)
    kb = kc * bc[..., None]
    kkt = jnp.einsum('nbhid,nbhjd->nbhij', kb, kc) * decay
    m = jnp.eye(CHUNK, dtype=kkt.dtype) + jnp.where(strict, kkt, 0.0)
    rhs = jnp.concatenate([vc * bc[..., None], kb * jnp.exp(gc)[..., None]], axis=-1)
    sol = lax.linalg.triangular_solve(m, rhs, left_side=True, lower=True, unit_diagonal=True)
    u, w = sol[..., :dv], sol[..., dv:]
    qk = jnp.einsum('nbhid,nbhjd->nbhij', qc, kc) * decay
    q_dec = qc * jnp.exp(gc)[..., None]
    g_last = gc[..., -1]
    k_dec = kc * jnp.exp(g_last[..., None] - gc)[..., None]

    def step(state, xs):
        u_n, w_n, q_n, qk_n, k_n, gl_n = xs
        v_new = u_n - jnp.einsum('bhcd,bhde->bhce', w_n, state)
        o = jnp.einsum('bhcd,bhde->bhce', q_n, state) + jnp.einsum('bhij,bhje->bhie', qk_n, v_new)
        state = state * jnp.exp(gl_n)[..., None, None] + jnp.einsum('bhcd,bhce->bhde', k_n, v_new)
        return state, o

    s0 = jnp.zeros((bsz, nh, dk, dv), dtype=u.dtype)
    _, o = lax.scan(step, s0, (u, w, q_dec, qk, k_dec, g_last))
    return o.transpose(1, 0, 3, 2, 4).reshape(bsz, s, nh, dv)


def gdn_group(qkv, z, b, a, conv_w, a_log, dt_bias, out_g):
    bsz, s, _ = qkv.shape
    qkv = jax.nn.silu(causal_depthwise_conv(qkv, conv_w))
    q, k, v = jnp.split(qkv, 3, axis=-1)
    shp = (bsz, s, GDN_HEADS, GDN_HEAD_DIM)
    q = l2norm(q.reshape(shp))
    k = l2norm(k.reshape(shp))
    v = v.reshape(shp)
    beta = jax.nn.sigmoid(b)
    g = -jnp.exp(a_log) * jax.nn.softplus(a + dt_bias)
    o = chunked_gated_delta_rule(q, k, v, beta, g)
    o = rmsnorm(o, out_g) * jax.nn.silu(z.reshape(shp))
    return o.reshape(bsz, s, D_GDN)


def expert_dispatch(xf, expert_id, gate, w_g, w_u, w_d):
    t, d = xf.shape
    n_assign = t * TOP_K
    flat_e = expert_id.reshape(-1)
    order = jnp.argsort(flat_e)
    sorted_e = flat_e[order]
    tok = order // TOP_K
    w_sorted = gate.reshape(-1)[order]
    counts = jax.ops.segment_sum(jnp.ones_like(flat_e), flat_e, num_segments=N_EXPERTS)
    padded = (counts + MOE_BLOCK - 1) // MOE_BLOCK * MOE_BLOCK
    pad_end = jnp.cumsum(padded)
    pad_start = pad_end - padded
    start = jnp.cumsum(counts) - counts
    dest = pad_start[sorted_e] + jnp.arange(n_assign, dtype=sorted_e.dtype) - start[sorted_e]
    n_blocks = -(-n_assign // MOE_BLOCK) + N_EXPERTS
    buf_tok = jnp.full((n_blocks * MOE_BLOCK,), t, dtype=tok.dtype).at[dest].set(tok)
    block_expert = jnp.minimum(
        jnp.searchsorted(pad_end, jnp.arange(n_blocks, dtype=pad_end.dtype) * MOE_BLOCK, side='right'),
        N_EXPERTS - 1)
    xpad = jnp.concatenate([xf, jnp.zeros((1, d), xf.dtype)], axis=0)
    xs = xpad[buf_tok].reshape(n_blocks, MOE_BLOCK, d)

    def expert_block(args):
        xb, e = args
        hb = jax.nn.silu(xb @ w_g[e]) * (xb @ w_u[e])
        return hb @ w_d[e]

    ys = lax.map(expert_block, (xs, block_expert)).reshape(n_blocks * MOE_BLOCK, d)
    return jax.ops.segment_sum(ys[dest] * w_sorted[:, None], tok, num_segments=t)


def hier_moe(xn, w_rg, b_rg, w_re, b_re, w_g, w_u, w_d):
    bsz, s, d = xn.shape
    xf = xn.reshape(bsz * s, d)
    rows = jnp.arange(bsz * s)
    group_logits = (xf @ w_rg + b_rg).astype(jnp.float32)
    g_sel = jnp.argmax(group_logits, axis=-1)
    p_group = jax.nn.softmax(group_logits, axis=-1)[rows, g_sel][:, None]
    e_logits = (xf @ w_re + b_re).astype(jnp.float32).reshape(-1, N_GROUPS, EXPERTS_PER_GROUP)
    in_group = e_logits[rows, g_sel]
    top_p, top_i = lax.top_k(jax.nn.softmax(in_group, axis=-1), TOP_K)
    gate = p_group * top_p / jnp.sum(top_p, axis=-1, keepdims=True)
    expert_id = g_sel[:, None] * EXPERTS_PER_GROUP + top_i
    y = expert_dispatch(xf, expert_id, gate, w_g, w_u, w_d)
    return y.reshape(bsz, s, d)


def setup_inputs(seed: int = 0) -> dict:
    key = jax.random.key(seed)
    ks = jax.random.split(key, 40)
    L = DEPTH

    def nrm(i, shape, scale):
        return jax.random.normal(ks[i], shape, jnp.float32) * scale

    def gain(i, shape):
        return 1.0 + nrm(i, shape, 0.02)

    a0 = jax.random.uniform(ks[10], (L, D_LRU), jnp.float32, 0.9, 0.999)
    s0 = a0 ** (1.0 / LRU_C)
    lru_lambda = jnp.log(s0) - jnp.log1p(-s0)
    gdn_a_log = jnp.log(jax.random.uniform(ks[13], (L, GDN_HEADS), jnp.float32, 1.0, 16.0))
    dt = jnp.exp(jax.random.uniform(ks[14], (L, GDN_HEADS), jnp.float32, float(np.log(1e-3)), float(np.log(1e-1))))
    gdn_dt_bias = dt + jnp.log(-jnp.expm1(-dt))
    return {
        'x': nrm(0, (BATCH, SEQ, D_MODEL), 1.0),
        'p': nrm(1, (L, BATCH, SEQ, D_PLE), 1.0),
        'norm_mix': gain(2, (L, D_MODEL)),
        'w_in': nrm(3, (L, D_MODEL, IN_COLS), D_MODEL ** -0.5),
        'lru_conv_w': nrm(4, (L, CONV_W, D_LRU), CONV_W ** -0.5),
        'lru_conv_b': nrm(5, (L, D_LRU), 0.02),
        'lru_wa': nrm(6, (L, LRU_BLOCKS, LRU_BLOCK_W, LRU_BLOCK_W), LRU_BLOCK_W ** -0.5),
        'lru_ba': nrm(7, (L, D_LRU), 0.02),
        'lru_wi': nrm(8, (L, LRU_BLOCKS, LRU_BLOCK_W, LRU_BLOCK_W), LRU_BLOCK_W ** -0.5),
        'lru_bi': nrm(9, (L, D_LRU), 0.02),
        'lru_lambda': lru_lambda,
        'lru_out_norm': gain(11, (L, D_LRU)),
        'gdn_conv_w': nrm(12, (L, CONV_W, 3 * D_GDN), CONV_W ** -0.5),
        'gdn_a_log': gdn_a_log,
        'gdn_dt_bias': gdn_dt_bias,
        'gdn_out_norm': gain(15, (L, GDN_HEAD_DIM)),
        'w_out': nrm(16, (L, D_MIX, D_MODEL), D_MIX ** -0.5),
        'norm_ffn': gain(17, (L, D_MODEL)),
        'w_router_group': nrm(18, (L, D_MODEL, N_GROUPS), D_MODEL ** -0.5),
        'b_router_group': nrm(19, (L, N_GROUPS), 0.01),
        'w_router_expert': nrm(20, (L, D_MODEL, N_EXPERTS), D_MODEL ** -0.5),
        'b_router_expert': nrm(21, (L, N_EXPERTS), 0.01),
        'w_exp_gate': nrm(22, (L, N_EXPERTS, D_MODEL, D_EXPERT), D_MODEL ** -0.5),
        'w_exp_up': nrm(23, (L, N_EXPERTS, D_MODEL, D_EXPERT), D_MODEL ** -0.5),
        'w_exp_down': nrm(24, (L, N_EXPERTS, D_EXPERT, D_MODEL), D_EXPERT ** -0.5),
        'norm_ple': gain(25, (L, D_MODEL)),
        'w_ple_gate': nrm(26, (L, D_MODEL, D_MODEL), D_MODEL ** -0.5),
        'w_ple': nrm(27, (L, D_PLE, D_MODEL), D_PLE ** -0.5),
        'norm_final': gain(28, (D_MODEL,)),
    }


def reference(x, p, norm_mix, w_in, lru_conv_w, lru_conv_b, lru_wa, lru_ba, lru_wi, lru_bi, lru_lambda,
              lru_out_norm, gdn_conv_w, gdn_a_log, gdn_dt_bias, gdn_out_norm, w_out, norm_ffn,
              w_router_group, b_router_group, w_router_expert, b_router_expert, w_exp_gate, w_exp_up,
              w_exp_down, norm_ple, w_ple_gate, w_ple, norm_final):
    h = x.astype(jnp.float32)
    o_gate = D_LRU
    o_qkv = 2 * D_LRU
    o_z = o_qkv + 3 * D_GDN
    o_beta = o_z + D_GDN
    o_alpha = o_beta + GDN_HEADS
    for l in range(DEPTH):
        xn = rmsnorm(h, norm_mix[l])
        proj = xn @ w_in[l]
        y_lru = rglru_group(proj[..., :o_gate], proj[..., o_gate:o_qkv], lru_conv_w[l], lru_conv_b[l],
                            lru_wa[l], lru_ba[l], lru_wi[l], lru_bi[l], lru_lambda[l], lru_out_norm[l])
        y_gdn = gdn_group(proj[..., o_qkv:o_z], proj[..., o_z:o_beta], proj[..., o_beta:o_alpha],
                          proj[..., o_alpha:], gdn_conv_w[l], gdn_a_log[l], gdn_dt_bias[l], gdn_out_norm[l])
        h = h + jnp.concatenate([y_lru, y_gdn], axis=-1) @ w_out[l]
        h = h + hier_moe(rmsnorm(h, norm_ffn[l]), w_router_group[l], b_router_group[l], w_router_expert[l],
                         b_router_expert[l], w_exp_gate[l], w_exp_up[l], w_exp_down[l])
        gate = jax.nn.sigmoid(rmsnorm(h, norm_ple[l]) @ w_ple_gate[l])
        h = h + (p[l].astype(jnp.float32) @ w_ple[l]) * gate
    return rmsnorm(h, norm_final).astype(x.dtype)
```

```python
import contextlib
import numpy as np
import concourse.bass as bass
import concourse.mybir as mybir
from concourse.bass_utils import run_bass_kernel_spmd

F32 = mybir.dt.float32
BF16 = mybir.dt.bfloat16
AF = mybir.ActivationFunctionType
ALU = mybir.AluOpType
AX = mybir.AxisListType

S = 4096
D = 1024
NCORES = 8
TC = 256
NSUB = TC // 128
NCH = S // TC
INC = 3080
EPS = 1e-6
GRP = 1024
TC2 = 512
SKIP = set()
SCHED = True
STOPAT = 99


class _Stop(Exception):
    pass


def _ck(k):
    if STOPAT <= k:
        raise _Stop()

SP = {}
_o = 0
for _n, _w in [("gmix", 8), ("g2", 8), ("g3", 8), ("gf", 8), ("lcw", 16), ("lcb", 4), ("ba", 4), ("bi", 4), ("lam", 4),
               ("lon", 4), ("gcw", 48), ("gg", 1), ("alog", 4), ("dtb", 4), ("br", 36)]:
    SP[_n] = (_o, _w)
    _o += _w
NSP = _o
C_ID, C_MU, C_MSU, C_ID4, C_TRI = 0, 128, 640, 1152, 1664
NCONST = 1792


class V:
    __slots__ = ("ap", "names")

    def __init__(self, ap, names):
        self.ap = ap
        self.names = names


class Buf:
    def __init__(self, t, name):
        self.t = t
        self.name = name

    def __getitem__(self, idx):
        return V(self.t[idx], (self.name,))

    def v(self, ap):
        return V(ap, (self.name,))


class Tok:
    __slots__ = ("sem", "val", "src")

    def __init__(self, sem, val, src):
        self.sem = sem
        self.val = val
        self.src = src


class Prog:
    def __init__(self, nc, ndsem=8):
        self.nc = nc
        self.eng = {"pe": nc.tensor, "dve": nc.vector, "act": nc.scalar, "pool": nc.gpsimd, "sp": nc.sync}
        self.sem = {k: nc.alloc_semaphore(f"s_{k}") for k in self.eng}
        self.cnt = {k: 0 for k in self.eng}
        self.dsem = {q: [nc.alloc_semaphore(f"d_{q}{i}") for i in range(ndsem)] for q in ("sp", "pool")}
        self.dcnt = {q: [0] * ndsem for q in self.dsem}
        self.drr = {q: 0 for q in self.dsem}
        self.waited = {k: {} for k in self.eng}
        self.lastw = {}
        self.readers = {}
        self.ninst = 0
        self.pend = None

    def _flush(self):
        if self.pend is not None:
            E, ins, tok = self.pend
            self.cnt[E] += 1
            tok.val = self.cnt[E]
            ins.then_inc(self.sem[E], 1)
            self.pend = None

    def _collect(self, E, reads, writes):
        toks = []
        for b in reads:
            t = self.lastw.get(b)
            if t is not None:
                toks.append(t)
        for b in writes:
            t = self.lastw.get(b)
            if t is not None:
                toks.append(t)
            toks.extend(self.readers.get(b, {}).values())
        return [t for t in toks if not (t.src == "pe" and E == "pe")]

    def _wait(self, E, toks):
        needs = {}
        for t in toks:
            key = id(t.sem)
            if self.waited[E].get(key, 0) >= t.val:
                continue
            if needs.get(key, (None, 0))[1] < t.val:
                needs[key] = (t.sem, t.val)
        for key, (sem, val) in needs.items():
            self.eng[E].wait_ge(sem, val)
            self.waited[E][key] = val
            self.ninst += 1

    def _commit(self, E, tok, reads, writes):
        for b in reads:
            self.readers.setdefault(b, {})[(E, id(tok.sem))] = tok
        for b in writes:
            self.lastw[b] = tok
            self.readers[b] = {}

    def begin(self):
        self.rec = []

    def schedule(self):
        rec, self.rec = self.rec, None
        n = len(rec)
        lastw, readers = {}, {}
        preds = [set() for _ in range(n)]
        for i, (kind, E, fn, reads, writes, cost) in enumerate(rec):
            psr = tuple(b for b in reads if b.startswith("ps"))
            rd = tuple(b for b in reads if not b.startswith("ps"))
            wr = tuple(writes) + psr
            for b in rd:
                if b in lastw:
                    preds[i].add(lastw[b])
            for b in wr:
                if b in lastw:
                    preds[i].add(lastw[b])
                preds[i].update(readers.get(b, ()))
            for b in rd:
                readers.setdefault(b, []).append(i)
            for b in wr:
                lastw[b] = i
                readers[b] = []
        succs = [[] for _ in range(n)]
        indeg = [0] * n
        for i in range(n):
            preds[i].discard(i)
            indeg[i] = len(preds[i])
            for p in preds[i]:
                succs[p].append(i)
        rt = [0.0] * n
        free = {}
        bl = [0.0] * n
        for i in range(n - 1, -1, -1):
            m = 0.0
            for sx in succs[i]:
                if bl[sx] > m:
                    m = bl[sx]
            bl[i] = rec[i][5] + 0.1 + m
        ready = [i for i in range(n) if indeg[i] == 0]
        while ready:
            best, bkey = None, None
            for i in ready:
                E = rec[i][1]
                key = (round(max(free.get(E, 0.0), rt[i]), 2), -bl[i], i)
                if bkey is None or key < bkey:
                    best, bkey = i, key
            ready.remove(best)
            kind, E, fn, reads, writes, cost = rec[best]
            start = bkey[0]
            if kind == "dma":
                free[E] = start + 0.15
                self.dma(E, fn[0], fn[1])
            else:
                free[E] = start + cost
                self.op(E, fn, reads, writes)
            fin = start + cost
            for sx in succs[best]:
                lat = 0.06 if rec[sx][1] == E else 0.12
                if rt[sx] < fin + lat:
                    rt[sx] = fin + lat
                indeg[sx] -= 1
                if indeg[sx] == 0:
                    ready.append(sx)

    def op(self, E, emit, reads=(), writes=(), cost=0.3):
        if getattr(self, "rec", None) is not None:
            self.rec.append(("op", E, emit, tuple(reads), tuple(writes), cost))
            return
        psr = tuple(b for b in reads if b.startswith("ps"))
        if psr:
            reads = tuple(b for b in reads if not b.startswith("ps"))
            writes = tuple(writes) + psr
        toks = self._collect(E, reads, writes)
        tok = None
        if self.pend is not None:
            if self.pend[0] == E and not any(t is self.pend[2] for t in toks):
                tok = self.pend[2]
                self.pend = None
            else:
                self._flush()
        self._wait(E, toks)
        ins = emit(self.eng[E])
        if tok is None:
            tok = Tok(self.sem[E], None, E)
        self.pend = (E, ins, tok)
        self._commit(E, tok, reads, writes)
        self.ninst += 1

    def dma(self, Q, out, in_):
        if getattr(self, "rec", None) is not None:
            nbytes = 4.0
            for d in out.ap.shape:
                nbytes *= d
            self.rec.append(("dma", Q, (out, in_), tuple(in_.names), tuple(out.names), 2.0 + nbytes / 150e3))
            return
        self._flush()
        i = self.drr[Q]
        self.drr[Q] = (i + 1) % len(self.dsem[Q])
        sem = self.dsem[Q][i]
        prev = self.dcnt[Q][i]
        key = id(sem)
        if prev and self.waited[Q].get(key, 0) < prev:
            self.eng[Q].wait_ge(sem, prev)
            self.waited[Q][key] = prev
        self._wait(Q, self._collect(Q, in_.names, out.names))
        ins = self.eng[Q].dma_start(out=out.ap, in_=in_.ap)
        self.dcnt[Q][i] += 16
        ins.then_inc(sem, 16)
        tok = Tok(sem, self.dcnt[Q][i], "dma")
        self._commit(Q, tok, in_.names, out.names)
        self.ninst += 1

    def barrier(self):
        self._flush()
        toks = [(self.sem[k], self.cnt[k]) for k in self.eng if self.cnt[k]]
        for q in self.dsem:
            toks += [(s, c) for s, c in zip(self.dsem[q], self.dcnt[q]) if c]
        for E in self.eng:
            for sem, val in toks:
                if self.waited[E].get(id(sem), 0) < val:
                    self.eng[E].wait_ge(sem, val)
                    self.waited[E][id(sem)] = val


def _ap(x):
    return x.ap if isinstance(x, V) else x


def _nm(*xs):
    r = ()
    for x in xs:
        if isinstance(x, V):
            r += x.names
    return r


def build(dbg=0):
    nc = bass.Bass("TRN2", target_bir_lowering=False)
    P = Prog(nc)
    global _NC, _P
    _NC, _P = nc, P

    def din(name, shape, dt=F32):
        return Buf(nc.dram_tensor(name, shape, dt, kind="ExternalInput").ap(), "dram_" + name)

    x_d = din("x", [S, D]); p_d = din("p", [S, 256]); sp_d = din("sp", [128, NSP]); cst_d = din("consts", [128, NCONST])
    win_d = din("w_in", [D, INC]); wab_d = din("wab", [8, 128, 128]); wout_d = din("w_out", [D, D]); wr_d = din("wr", [D, 36])
    wg_d = din("wg", [32, D, 512]); wu_d = din("wu", [32, D, 512]); wd_d = din("wd", [32, 512, D])
    wpg_d = din("w_pg", [D, D]); wple_d = din("w_ple", [256, D])
    out_d = Buf(nc.dram_tensor("out", [S, D], F32, kind="ExternalOutput").ap(), "dram_out")
    skind = "ExternalOutput" if dbg else "Internal"
    h1s_d = Buf(nc.dram_tensor("h1s", [8, 128, S], F32, kind=skind).ap(), "dram_h1s")
    xn2s_d = Buf(nc.dram_tensor("xn2s", [8, 128, S], BF16, kind="Internal").ap(), "dram_xn2s")
    if dbg:
        ymix_d = Buf(nc.dram_tensor("dbg_ymix", [8, 128, S], F32, kind="ExternalOutput").ap(), "dram_ymix")
        g_d = Buf(nc.dram_tensor("dbg_g", [128, 32, 32], F32, kind="ExternalOutput").ap(), "dram_g")

    es = contextlib.ExitStack()
    nb = [0]

    sbc = {}

    def sb(stack, name, shape, dt=F32):
        key = (name, tuple(shape), str(dt))
        if sbc.get("on") and key in sbc:
            return sbc[key]
        if sbc.get("on"):
            sbc[key] = sb_(stack, name, shape, dt)
            return sbc[key]
        return sb_(stack, name, shape, dt)

    def sb_(stack, name, shape, dt=F32):
        nb[0] += 1
        t = stack.enter_context(nc.sbuf_tensor(f"{name}_{nb[0]}", shape, dt))
        return Buf(t, f"{name}_{nb[0]}")

    ps = [Buf(es.enter_context(nc.psum_tensor(f"ps{i}", [128, 512], F32)), f"ps{i}") for i in range(8)]
    psi = [0]

    psbanks = [list(range(8))]

    def nps():
        psi[0] = (psi[0] + 1) % len(psbanks[0])
        return ps[psbanks[0][psi[0]]]

    def fsz(v):
        n = 1
        for d in v.ap.shape[1:]:
            n *= d
        return n

    def mm(out, lhsT, rhs, start=True, stop=True):
        c = 0.03 + fsz(out) / 2400.0 * (4 if lhsT.ap.dtype == F32 else 1)
        P.op("pe", lambda e: e.matmul(out.ap, lhsT.ap, rhs.ap, start=start, stop=stop),
             reads=_nm(lhsT, rhs), writes=out.names, cost=c)

    def tr(out, in_, ident):
        P.op("pe", lambda e: e.transpose(out.ap, in_.ap, ident.ap), reads=_nm(in_, ident), writes=out.names,
             cost=0.03 + fsz(out) / 600.0)

    def act(out, in_, func, scale=1.0, bias=0.0, accum=None):
        kw = {}
        if accum is not None:
            kw["accum_out"] = accum.ap
        P.op("act", lambda e: e.activation(out.ap, in_.ap, func, bias=_ap(bias), scale=_ap(scale), **kw),
             reads=_nm(in_, scale, bias), writes=_nm(out, accum), cost=0.25 + fsz(out) / 1200.0)

    def tt(out, a, b, op, eng="dve"):
        P.op(eng, lambda e: e.tensor_tensor(out.ap, a.ap, b.ap, op), reads=_nm(a, b), writes=out.names,
             cost=(0.12 + fsz(out) / 960.0) * (2.2 if eng == "pool" else 1.0))

    def ts(out, a, s1, s2, op0, op1=None, eng="dve"):
        c = (0.12 + fsz(out) / 960.0) * (2.2 if eng == "pool" else 1.0)
        if op1 is None:
            P.op(eng, lambda e: e.tensor_scalar(out.ap, a.ap, _ap(s1), None, op0), reads=_nm(a, s1), writes=out.names, cost=c)
        else:
            P.op(eng, lambda e: e.tensor_scalar(out.ap, a.ap, _ap(s1), _ap(s2), op0, op1), reads=_nm(a, s1, s2), writes=out.names, cost=c)

    def stt(out, a, s, b, op0, op1, eng="dve"):
        P.op(eng, lambda e: e.scalar_tensor_tensor(out.ap, a.ap, _ap(s), b.ap, op0, op1), reads=_nm(a, s, b), writes=out.names,
             cost=(0.12 + fsz(out) / 960.0) * (2.2 if eng == "pool" else 1.0))

    def cp(out, in_, eng="act"):
        if eng == "act":
            P.op("act", lambda e: e.activation(out.ap, in_.ap, AF.Copy), reads=in_.names, writes=out.names,
                 cost=0.25 + fsz(out) / 1200.0)
        else:
            P.op(eng, lambda e: e.tensor_copy(out.ap, in_.ap), reads=in_.names, writes=out.names,
                 cost=(0.12 + fsz(out) / 960.0) * (2.2 if eng == "pool" else 1.0))

    def recip(out, in_):
        P.op("dve", lambda e: e.reciprocal(out.ap, in_.ap), reads=in_.names, writes=out.names, cost=0.12 + fsz(out) / 960.0)

    def memset(out, val, eng="pool"):
        P.op(eng, lambda e: e.memset(out.ap, val), writes=out.names)

    def rsqrt_from(out, in_ps, scale):
        act(out, in_ps, AF.Ln, scale=scale, bias=EPS)
        act(out, out, AF.Exp, scale=-0.5)

    def sigm(out, in_, scale=1.0, nbias=0.0):
        act(out, in_, AF.Exp, scale=-scale, bias=nbias)
        act(out, out, AF.Ln, bias=1.0)
        act(out, out, AF.Exp, scale=-1.0)

    cst = sb(es, "cst", [128, NCONST]); spt = sb(es, "sp", [128, NSP])
    P.dma("sp", cst[:], cst_d[:]); P.dma("sp", spt[:], sp_d[:])
    ident = cst[:, C_ID:C_ID + 128]
    maskU4 = cst[:, C_MU:C_MU + 512]; maskSU4 = cst[:, C_MSU:C_MSU + 512]; ident4 = cst[:, C_ID4:C_ID4 + 512]
    triU = cst[:, C_TRI:C_TRI + 128]

    def spc(name, i=0, n=1):
        o, w = SP[name]
        return spt[:, o + i:o + i + n]

    ones_f = sb(es, "ones_f", [128, 128]); ones_b = sb(es, "ones_b", [128, 128], BF16)
    memset(ones_f[:], 1.0); memset(ones_b[:], 1.0)
    Gtok = sb(es, "Gtok", [128, 32, 32])
    der = sb(es, "der", [128, 24])
    lam = spc("lam", 0, 4)
    act(der[:, 12:16], lam, AF.Exp, scale=-1.0)
    act(der[:, 12:16], der[:, 12:16], AF.Ln, bias=1.0)
    ts(der[:, 0:4], der[:, 12:16], -8.0, None, ALU.mult)
    ts(der[:, 4:8], der[:, 12:16], -16.0, None, ALU.mult)
    act(der[:, 12:16], spc("alog", 0, 4), AF.Exp)
    ts(der[:, 8:12], der[:, 12:16], -1.0, None, ALU.mult)
    negA = der[:, 8:12]
    ts(der[:, 16:20], spc("ba", 0, 4), -1.0, None, ALU.mult)
    ts(der[:, 20:24], spc("bi", 0, 4), -1.0, None, ALU.mult)

    s1 = contextlib.ExitStack()
    wba = sb(s1, "wba", [128, 8, 8], BF16)
    P.dma("pool", wba[:], win_d.v(win_d.t[:, 3072:3080].rearrange("(kt p) c -> p kt c", p=128)))
    wst = [sb(s1, f"wst{i}", [128, 8, 256], BF16) for i in range(3)]
    wrr = [0]
    wab = sb(s1, "wab", [128, 8, 128], BF16)
    P.dma("pool", wab[:], wab_d.v(wab_d.t.rearrange("n p c -> p n c")))
    wr = sb(s1, "wr", [128, 8, 36])
    P.dma("sp", wr[:], wr_d.v(wr_d.t.rearrange("(kt p) c -> p kt c", p=128)))
    wout = sb(s1, "wout", [128, 8, D], BF16)
    for q in range(4):
        P.dma("pool", wout[:, 2 * q:2 * q + 2, :], wout_d.v(wout_d.t[256 * q:256 * (q + 1), :].rearrange("(kt p) c -> p kt c", p=128)))
    NT = TC // 128
    xtokP = [sb(s1, f"xtok{i}", [128, NT, D]) for i in range(2)]
    xTP = [sb(s1, f"xT{i}", [128, 8, TC]) for i in range(2)]
    sqP = [sb(s1, f"sq{i}", [128, 8, TC], BF16) for i in range(2)]
    xnTP = [sb(s1, f"xnT{i}", [128, 8, TC], BF16) for i in range(2)]
    rstdP = [sb(s1, f"rstd{i}", [128, TC]) for i in range(2)]
    ymixP = [sb(s1, f"ymix{i}", [128, 8, TC], BF16) for i in range(2)]
    halo = sb(s1, "halo", [128, 16, 4]); memset(halo[:], 0.0)
    hst = sb(s1, "hst", [128, 4]); memset(hst[:], 0.0)
    Sst = sb(s1, "Sst", [128, 512]); memset(Sst[:], 0.0)
    Sb = sb(s1, "Sb", [128, 512], BF16); memset(Sb[:], 0.0)
    xn2b = sb(s1, "xn2b", [128, 8, TC], BF16)

    def conv(pz, hidx, cb, xc, wname, widx, bias=None, eng="dve"):
        cp(cb[:, 0:3], halo[:, hidx, 0:3], eng=eng)
        cp(cb[:, 3:TC + 3], pz[:, 0:TC])
        cp(halo[:, hidx, 0:3], cb[:, TC:TC + 3], eng=eng)
        w = lambda j: spc(wname, widx * 4 + j)
        if bias is None:
            ts(xc, cb[:, 0:TC], w(0), None, ALU.mult, eng=eng)
        else:
            ts(xc, cb[:, 0:TC], w(0), bias, ALU.mult, ALU.add, eng=eng)
        for j in range(1, 4):
            stt(xc, cb[:, j:j + TC], w(j), xc, ALU.mult, ALU.add, eng=eng)

    sbc["on"] = True

    def body(c):
        t0 = c * TC
        par = c % 2
        psbanks[0] = [0, 1, 2]
        xtok = xtokP[par]; xT = xTP[par]; sq = sqP[par]; xnT = xnTP[par]; rstd = rstdP[par]; ymix = ymixP[par]
        wcache = {}

        def proj(col0, width=128):
            grp = col0 // 256
            if grp not in wcache:
                wb = wst[wrr[0] % 3]
                wrr[0] += 1
                P.dma("pool", wb[:], win_d.v(win_d.t[:, 256 * grp:256 * (grp + 1)].rearrange("(kt p) c -> p kt c", p=128)))
                wcache[grp] = wb
            wb = wcache[grp]
            off = col0 - 256 * grp
            pz = nps()
            for kt in range(8):
                mm(pz[:width, 0:TC], wb[:, kt, off:off + width], xnT[:, kt, :], start=(kt == 0), stop=(kt == 7))
            return pz

        P.dma("sp", xtok[:], x_d.v(x_d.t[t0:t0 + TC, :].rearrange("(n p) d -> p n d", p=128)))
        for dt in range(8):
            pz = nps()
            for n in range(NT):
                tr(pz[:, n * 128:(n + 1) * 128], xtok[:, n, dt * 128:(dt + 1) * 128], ident)
            cp(xT[:, dt, :], pz[:, 0:TC], eng=("act" if dt % 2 else "dve"))
        act(sq[:], xT[:], AF.Square)
        pz = nps()
        for dt in range(8):
            mm(pz[:, 0:TC], ones_b[:], sq[:, dt, :], start=(dt == 0), stop=(dt == 7))
        rsqrt_from(rstd[:], pz[:, 0:TC], 1.0 / D)
        for dt in range(8):
            stt(xnT[:, dt, :], xT[:, dt, :], spc("gmix", dt), rstd[:], ALU.mult, ALU.mult)

        for sl in (s1,):
          if 'lru' not in SKIP:
                T = lambda n, dt=F32, w=TC: sb(sl, "L" + n, [128, w], dt)
                cbL = [T("cb0", F32, TC + 3), T("cb1", F32, TC + 3)]; xcL = [T("xc0"), T("xc1")]; xcbL = [T("xcb0", BF16), T("xcb1", BF16)]
                r = T("r"); ig = T("ig"); a = T("a"); s_ = T("s")
                hb = T("hb"); gb = T("gb"); tmp = T("tmp")
                yl = sb(sl, "yl", [128, 4, TC]); ysq = sb(sl, "ysq", [128, 4, TC], BF16); rs = T("rs")
                for ct in range(4):
                    cb = cbL[ct % 2]; xc = xcL[ct % 2]; xcb = xcbL[ct % 2]
                    pz = proj(ct * 128)
                    conv(pz, ct, cb, xc[:], "lcw", ct, bias=spc("lcb", ct))
                    cp(xcb[:], xc[:])
                    pr = nps(); mm(pr[:, 0:TC], wab[:, ct, :], xcb[:])
                    sigm(r[:], pr[:, 0:TC], nbias=der[:, 16 + ct:17 + ct])
                    pi = nps(); mm(pi[:, 0:TC], wab[:, 4 + ct, :], xcb[:])
                    sigm(ig[:], pi[:, 0:TC], nbias=der[:, 20 + ct:21 + ct])
                    act(a[:], r[:], AF.Exp, scale=der[:, ct:ct + 1])
                    act(s_[:], r[:], AF.Exp, scale=der[:, 4 + ct:5 + ct])
                    act(s_[:], s_[:], AF.Ln, scale=-1.0, bias=1.0)
                    act(s_[:], s_[:], AF.Exp, scale=0.5)
                    tt(s_[:], s_[:], ig[:], ALU.mult, eng="pool")
                    tt(s_[:], s_[:], xc[:], ALU.mult, eng="pool")
                    P.op("dve", lambda e, ct=ct: e.tensor_tensor_scan(hb.t[:], a.t[:], s_.t[:], hst.t[:, ct:ct + 1], ALU.mult, ALU.add),
                         reads=(a.name, s_.name, hst.name), writes=(hb.name,))
                    cp(hst[:, ct:ct + 1], hb[:, TC - 1:TC], eng="dve")
                    pg = proj(512 + ct * 128)
                    cp(gb[:], pg[:, 0:TC])
                    tt(tmp[:], gb[:], gb[:], ALU.mult, eng="pool")
                    ts(tmp[:], tmp[:], 0.044715, 1.0, ALU.mult, ALU.add)
                    tt(tmp[:], tmp[:], gb[:], ALU.mult)
                    sigm(tmp[:], tmp[:], scale=1.5957691216057308)
                    tt(tmp[:], tmp[:], gb[:], ALU.mult)
                    tt(yl[:, ct, :], hb[:], tmp[:], ALU.mult, eng="pool")
                    act(ysq[:, ct, :], yl[:, ct, :], AF.Square)
                pz = nps()
                for ct in range(4):
                    mm(pz[:, 0:TC], ones_b[:], ysq[:, ct, :], start=(ct == 0), stop=(ct == 3))
                rsqrt_from(rs[:], pz[:, 0:TC], 1.0 / 512)
                for ct in range(4):
                    stt(ymix[:, ct, :], yl[:, ct, :], spc("lon", ct), rs[:], ALU.mult, ALU.mult)

        for sg in (s1,):
          if 'gdn' not in SKIP:
                T = lambda n, dt=F32, w=512: sb(sg, n, [128, w], dt)
                cbs = [T(f"cb{i}", F32, TC + 3) for i in range(3)]; xcs = [T(f"xc{i}", F32, TC) for i in range(3)]
                qn = sb(sg, f"qn{par}", [128, 4, TC]); kn = sb(sg, f"kn{par}", [128, 4, TC]); vT = sb(sg, f"vT{par}", [128, 4, TC])
                zs = sb(sg, f"zs{par}", [128, 4, TC], BF16)
                rns = [T("rn0", F32, TC), T("rn1", F32, TC)]
                for tile in range(12):
                    pz = proj(1024 + tile * 128)
                    cb = cbs[tile % 3]; xc = xcs[tile % 3]
                    conv(pz, 4 + tile, cb, xc[:], "gcw", tile)
                    dst = (qn, kn, vT)[tile // 4]
                    sigm(dst[:, tile % 4, :], xc[:])
                    tt(dst[:, tile % 4, :], dst[:, tile % 4, :], xc[:], ALU.mult, eng="pool")
                for h in range(4):
                    pz = proj(2560 + h * 128)
                    rn = rns[h % 2]
                    sigm(rn[:], pz[:, 0:TC])
                    tt(zs[:, h, :], rn[:], pz[:, 0:TC], ALU.mult)
                for which, dst in ((0, qn), (1, kn)):
                    act(sq[:, 0:4, :], dst[:], AF.Square)
                    for h in range(4):
                        pz = nps()
                        rn = rns[h % 2]
                        mm(pz[:, 0:TC], ones_b[:], sq[:, h, :])
                        rsqrt_from(rn[:], pz[:, 0:TC], 1.0)
                        if which == 0:
                            stt(dst[:, h, :], dst[:, h, :], 128.0 ** -0.5, rn[:], ALU.mult, ALU.mult)
                        else:
                            tt(dst[:, h, :], dst[:, h, :], rn[:], ALU.mult)
                yield
                psbanks[0] = [3, 4, 5, 6, 7]
                bal = sb(sg, "bal", [128, 8]); beta = sb(sg, "beta", [128, 4]); g_ = sb(sg, "g", [128, 4]); gcl = sb(sg, "gcl", [128, 8])
                sc8 = sb(sg, "sc8", [128, 24])
                dg = sb(sg, "dg", [128, 8, 128]); egcR = T("egcR"); arg = T("arg"); dT = T("dT"); Dm = T("Dm"); DmS = T("DmS")
                NB = T("NB"); Pk = [T("P0"), T("P1")]; Lk = [T("L0"), T("L1")]; X = T("X"); QK = T("QK", BF16); ATb = T("ATb", BF16)
                Kbg = T("Kbg", BF16); kdec = T("kdec", BF16); Vb = T("Vb", BF16); U = T("U"); WT = T("WT", BF16); qdT = T("qdT", BF16)
                vnew = T("vnew", BF16); o_ = T("o"); osq = T("osq", BF16); rs = T("rs")
                H = lambda h: slice(h * 128, (h + 1) * 128)
                for sc in range(NSUB if 'gdn_sub' not in SKIP else 0):
                    tk = slice(sc * 128, (sc + 1) * 128)
                    pz = nps()
                    for kt in range(8):
                        mm(pz[:, 0:8], xnT[:, kt, tk], wba[:, kt, :], start=(kt == 0), stop=(kt == 7))
                    cp(bal[:], pz[:, 0:8])
                    sigm(beta[:], bal[:, 0:4])
                    tt(g_[:], bal[:, 4:8], spc("dtb", 0, 4), ALU.add)
                    act(g_[:], g_[:], AF.Exp)
                    act(g_[:], g_[:], AF.Ln, bias=1.0)
                    tt(g_[:], g_[:], negA, ALU.mult)
                    _ck(1)
                    pz = nps()
                    mm(pz[:, 0:4], triU, g_[:]); mm(pz[:, 4:8], ones_f[:], g_[:])
                    cp(gcl[:], pz[:, 0:8])
                    gc = lambda h: gcl[:, h:h + 1]
                    ts(sc8[:, 0:4], gcl[:, 0:4], -1.0, None, ALU.mult)
                    act(sc8[:, 4:8], gcl[:, 0:4], AF.Exp)
                    tt(sc8[:, 8:12], gcl[:, 4:8], gcl[:, 0:4], ALU.subtract)
                    act(sc8[:, 8:12], sc8[:, 8:12], AF.Exp)
                    tt(sc8[:, 12:16], beta[:], sc8[:, 4:8], ALU.mult)
                    act(sc8[:, 16:20], gcl[:, 4:8], AF.Exp)
                    _ck(2)
                    for h in range(4):
                        ts(dg[:, h, :], ident, gcl[:, h:h + 1], None, ALU.mult)
                        ts(dg[:, 4 + h, :], ident, beta[:, h:h + 1], None, ALU.mult)
                    pR1 = nps(); mm(pR1[:], ones_f[:], dg[:, 0:4, :])
                    pR2 = nps(); mm(pR2[:], ones_f[:], dg[:, 4:8, :])
                    _ck(3)
                    act(egcR[:], pR1[:], AF.Exp)
                    for h in range(4):
                        ts(arg[:, H(h)], pR1[:, H(h)], sc8[:, h:h + 1], 0.0, ALU.add, ALU.min)
                    act(dT[:], arg[:], AF.Exp)
                    tt(Dm[:], dT[:], maskU4, ALU.mult, eng="pool")
                    tt(DmS[:], dT[:], maskSU4, ALU.mult, eng="pool")
                    tt(NB[:], DmS[:], pR2[:], ALU.mult)
                    _ck(4)
                    pK = nps(); pQ = nps()
                    for h in range(4):
                        mm(pK[:, H(h)], kn[:, h, tk], kn[:, h, tk])
                    for h in range(4):
                        mm(pQ[:, H(h)], kn[:, h, tk], qn[:, h, tk])
                    tt(Pk[0][:], pK[:], NB[:], ALU.mult)
                    tt(QK[:], pQ[:], Dm[:], ALU.mult)
                    _ck(5)
                    pT = nps()
                    for h in range(4):
                        tr(pT[:, H(h)], Pk[0][:, H(h)], ident)
                    cp(Lk[0][:], pT[:])
                    tt(X[:], ident4, Pk[0][:], ALU.subtract)
                    _ck(6)
                    cur = 0
                    for lev in range(1, 7):
                        nxt = 1 - cur
                        pL = nps()
                        if lev < 6:
                            pP = nps()
                            for h in range(4):
                                mm(pP[:, H(h)], Lk[cur][:, H(h)], Pk[cur][:, H(h)])
                            cp(Pk[nxt][:], pP[:])
                            for h in range(4):
                                tr(pL[:, H(h)], Pk[nxt][:, H(h)], ident)
                        else:
                            for h in range(4):
                                mm(pL[:, H(h)], Pk[cur][:, H(h)], Lk[cur][:, H(h)])
                        cp(Lk[nxt][:], pL[:])
                        pX = nps()
                        for h in range(4):
                            mm(pX[:, H(h)], Lk[nxt][:, H(h)], X[:, H(h)])
                        tt(X[:], X[:], pX[:], ALU.add)
                        cur = nxt
                    cp(ATb[:], X[:])
                    _ck(7)
                    pKt = nps(); pVt = nps()
                    for h in range(4):
                        tr(pKt[:, H(h)], kn[:, h, tk], ident)
                    for h in range(4):
                        tr(pVt[:, H(h)], vT[:, h, tk], ident)
                    for h in range(4):
                        ts(Kbg[:, H(h)], pKt[:, H(h)], sc8[:, 12 + h:13 + h], None, ALU.mult)
                        ts(kdec[:, H(h)], pKt[:, H(h)], sc8[:, 8 + h:9 + h], None, ALU.mult)
                        ts(Vb[:, H(h)], pVt[:, H(h)], beta[:, h:h + 1], None, ALU.mult)
                    _ck(8)
                    pU = nps(); pW = nps()
                    for h in range(4):
                        mm(pU[:, H(h)], ATb[:, H(h)], Vb[:, H(h)])
                    for h in range(4):
                        mm(pW[:, H(h)], Kbg[:, H(h)], ATb[:, H(h)])
                    cp(U[:], pU[:])
                    cp(WT[:], pW[:], eng="dve")
                    _ck(9)
                    tt(qdT.v(qdT.t[:].rearrange("p (h c) -> p h c", h=4)), qn[:, :, tk],
                       egcR.v(egcR.t[:].rearrange("p (h c) -> p h c", h=4)), ALU.mult)
                    pWS = nps()
                    for h in range(4):
                        mm(pWS[:, H(h)], WT[:, H(h)], Sb[:, H(h)])
                    tt(vnew[:], U[:], pWS[:], ALU.subtract)
                    pO = nps()
                    for h in range(4):
                        mm(pO[:, H(h)], Sb[:, H(h)], qdT[:, H(h)], start=True, stop=False)
                        mm(pO[:, H(h)], vnew[:, H(h)], QK[:, H(h)], start=False, stop=True)
                    pS = nps()
                    for h in range(4):
                        mm(pS[:, H(h)], kdec[:, H(h)], vnew[:, H(h)])
                    for h in range(4):
                        stt(Sst[:, H(h)], Sst[:, H(h)], sc8[:, 16 + h:17 + h], pS[:, H(h)], ALU.mult, ALU.add)
                    cp(Sb[:], Sst[:])
                    _ck(10)
                    cp(o_[:], pO[:], eng="dve")
                    act(osq[:], pO[:], AF.Square)
                    pN = nps(); mm(pN[:], ones_b[:], osq[:])
                    rsqrt_from(rs[:], pN[:], 1.0 / 128)
                    tt(o_[:], o_[:], rs[:], ALU.mult)
                    stt(ymix[:, 4:8, tk], o_.v(o_.t[:].rearrange("p (h c) -> p h c", h=4)), spc("gg", 0), zs[:, :, tk], ALU.mult, ALU.mult)

        if dbg:
            for kt in range(8):
                P.dma("pool", ymix_d.v(ymix_d.t[kt, :, t0:t0 + TC]), ymix[:, kt, :])
        for dt in range(8):
            pz = nps()
            for kt in range(8):
                mm(pz[:, 0:TC], wout[:, kt, dt * 128:(dt + 1) * 128], ymix[:, kt, :], start=(kt == 0), stop=(kt == 7))
            tt(xT[:, dt, :], xT[:, dt, :], pz[:, 0:TC], ALU.add)
        act(sq[:], xT[:], AF.Square)
        pz = nps()
        for dt in range(8):
            mm(pz[:, 0:TC], ones_b[:], sq[:, dt, :], start=(dt == 0), stop=(dt == 7))
        rsqrt_from(rstd[:], pz[:, 0:TC], 1.0 / D)
        xn2f = xtok.v(xtok.t[:].rearrange("p n d -> p (n d)").rearrange("p (k t) -> p k t", k=8))
        xn2f_k = lambda kt, sl: xtok.v(xtok.t[:].rearrange("p n d -> p (n d)").rearrange("p (k t) -> p k t", k=8)[:, kt, sl])
        for dt in range(8):
            stt(xn2f_k(dt, slice(0, TC)), xT[:, dt, :], spc("g2", dt), rstd[:], ALU.mult, ALU.mult)
        cp(xn2b[:], xn2f)
        for sr in (s1,):
            R = lambda n, w: sb(sr, n, [128, w])
            lg = R("lg", 36); gmax = R("gmax", 2); oh = R("oh", 4); eg = R("eg", 4); sume = R("sume", 2); pen = R("pen", 4)
            ml = R("ml", 32); m8 = R("m8", 8); is1 = R("is1", 32); is2 = R("is2", 32); w12 = R("w12", 4)
            for n in range(NT):
                tkn = slice(n * 128, (n + 1) * 128)
                pz = nps()
                for kt in range(8):
                    mm(pz[:, 0:36], xn2f_k(kt, tkn), wr[:, kt, :], start=(kt == 0), stop=(kt == 7))
                tt(lg[:], pz[:, 0:36], spc("br", 0, 36), ALU.add)
                P.op("dve", lambda e: e.reduce_max(gmax.t[:, 0:1], lg.t[:, 0:4], AX.X), reads=(lg.name,), writes=(gmax.name,))
                ts(oh[:], lg[:, 0:4], gmax[:, 0:1], None, ALU.is_equal)
                ts(gmax[:, 1:2], gmax[:, 0:1], -1.0, None, ALU.mult)
                act(eg[:], lg[:, 0:4], AF.Exp, bias=gmax[:, 1:2], accum=sume[:, 0:1])
                recip(sume[:, 1:2], sume[:, 0:1])
                ts(pen[:], oh[:], 1e30, -1e30, ALU.mult, ALU.add)
                tt(ml.v(ml.t[:].rearrange("p (g j) -> p g j", g=4)), lg.v(lg.t[:, 4:36].rearrange("p (g j) -> p g j", g=4)),
                   pen.v(pen.t[:].unsqueeze(2).to_broadcast([128, 4, 8])), ALU.add)
                P.op("dve", lambda e: e.max(m8.t[:], ml.t[:]), reads=(ml.name,), writes=(m8.name,))
                ts(is1[:], ml[:], m8[:, 0:1], None, ALU.is_equal)
                ts(is2[:], ml[:], m8[:, 1:2], None, ALU.is_equal)
                tt(w12[:, 0:1], m8[:, 1:2], m8[:, 0:1], ALU.subtract)
                act(w12[:, 0:1], w12[:, 0:1], AF.Exp)
                ts(w12[:, 1:2], w12[:, 0:1], 1.0, None, ALU.add)
                recip(w12[:, 1:2], w12[:, 1:2])
                tt(w12[:, 2:3], w12[:, 1:2], sume[:, 1:2], ALU.mult)
                tt(w12[:, 3:4], w12[:, 2:3], w12[:, 0:1], ALU.mult)
                tile_i = c * NT + n
                ts(is1[:], is1[:], w12[:, 2:3], None, ALU.mult)
                stt(Gtok[:, tile_i, :], is2[:], w12[:, 3:4], is1[:], ALU.mult, ALU.add)
        P.dma("sp", h1s_d.v(h1s_d.t.rearrange("d p t -> p d t")[:, :, t0:t0 + TC]), xT[:])
        P.dma("sp", xn2s_d.v(xn2s_d.t.rearrange("d p t -> p d t")[:, :, t0:t0 + TC]), xn2b[:])

    nch = NCH if dbg != 9 else 2
    gens = {0: body(0)}
    P.begin(); next(gens[0]); P.schedule()
    for c in range(nch):
        P.begin()
        for _ in gens.pop(c):
            pass
        if c + 1 < nch:
            gens[c + 1] = body(c + 1)
            next(gens[c + 1])
        P.schedule()
    sbc["on"] = False
    global _SB_LEFT
    _SB_LEFT = nc.sbuf_bytes_remaining
    if dbg:
        P.dma("sp", g_d[:], Gtok[:])
    P.barrier()
    s1.close()

    if dbg in (1, 9):
        P.barrier()
        es.close()
        return nc

    s2 = contextlib.ExitStack()
    accs = [sb(s2, "accA", [128, 8, GRP]), sb(s2, "accB", [128, 8, GRP])]
    cur = [accs[0]]
    xn2 = sb(s2, "xn2", [128, 8, GRP], BF16)
    wgb = [sb(s2, f"wg{i}", [128, 8, 512], BF16) for i in range(2)]
    wub = [sb(s2, f"wu{i}", [128, 8, 512], BF16) for i in range(2)]
    wdb = [sb(s2, f"wd{i}", [128, 4, D], BF16) for i in range(2)]
    GT = sb(s2, "GT", [32, GRP])
    sel = sb(s2, "sel", [32, 128])
    gs = sb(s2, "gs", [128, TC2]); sgT = [sb(s2, f"sg{i}", [128, TC2]) for i in range(2)]
    hT = [sb(s2, f"hT{i}", [128, 4, TC2], BF16) for i in range(2)]
    wpg = sb(s2, "wpg", [128, 8, D], BF16); wple = sb(s2, "wple", [128, 2, D], BF16)
    for q in range(4):
        P.dma("pool", wpg[:, 2 * q:2 * q + 2, :], wpg_d.v(wpg_d.t[256 * q:256 * (q + 1), :].rearrange("(kt p) c -> p kt c", p=128)))
    P.dma("pool", wple[:], wple_d.v(wple_d.t.rearrange("(kt p) c -> p kt c", p=128)))
    rs3 = sb(s2, "rs3", [128, TC2]); sq3 = sb(s2, "sq3", [128, 8, TC2], BF16); xn3 = sq3
    ptok = sb(s2, "ptok", [128, 4, 256]); pT = sb(s2, "pT", [128, 2, TC2], BF16)
    sg3 = sb(s2, "sg3", [128, TC2]); otok = sb(s2, "otok", [128, 2, D])

    def load_gu(i):
        e = i % 32
        b = i % 2
        for q in range(4):
            P.dma("pool", wgb[b][:, 2 * q:2 * q + 2, :], wg_d.v(wg_d.t[e, 256 * q:256 * (q + 1), :].rearrange("(kt p) f -> p kt f", p=128)))
            P.dma("pool", wub[b][:, 2 * q:2 * q + 2, :], wu_d.v(wu_d.t[e, 256 * q:256 * (q + 1), :].rearrange("(kt p) f -> p kt f", p=128)))

    def load_d(i):
        for q in range(2):
            P.dma("pool", wdb[i % 2][:, 2 * q:2 * q + 2, :], wd_d.v(wd_d.t[i % 32, 256 * q:256 * (q + 1), :].rearrange("(ft p) d -> p ft d", p=128)))

    def accv(d, tk):
        return V(cur[0].t[:, d, tk], (f"{cur[0].name}#{d}",))

    def accall(tk):
        return V(cur[0].t[:, :, tk], tuple(f"{cur[0].name}#{d}" for d in range(8)))

    def replay(ops):
        for kind, E, fn, reads, writes, cost in ops:
            if kind == "dma":
                P.dma(E, fn[0], fn[1])
            else:
                P.op(E, fn, reads, writes)

    def emit_gu(b, tk, hb_):
        pgs = nps(); mm(pgs[:], sel[:], GT[:, tk])
        cp(gs[:], pgs[:])
        for f in range(4):
            pg_ = nps(); pu_ = nps()
            for kt in range(8):
                mm(pg_[:], wgb[b][:, kt, f * 128:(f + 1) * 128], xn2[:, kt, tk], start=(kt == 0), stop=(kt == 7))
            for kt in range(8):
                mm(pu_[:], wub[b][:, kt, f * 128:(f + 1) * 128], xn2[:, kt, tk], start=(kt == 0), stop=(kt == 7))
            st = sgT[f % 2]
            act(st[:], pg_[:], AF.Silu)
            tt(st[:], st[:], pu_[:], ALU.mult)
            tt(hb_[:, f, :], st[:], gs[:], ALU.mult)

    def emit_d(b, tk, hb_):
        for d in range(8):
            pd_ = nps()
            for f in range(4):
                mm(pd_[:], wdb[b][:, f, d * 128:(d + 1) * 128], hb_[:, f, :], start=(f == 0), stop=(f == 3))
            tt(accv(d, tk), accv(d, tk), pd_[:], ALU.add)

    NG = S // GRP
    load_gu(0); load_d(0)
    it = 0
    un = 0
    deferred = []
    for g in range(NG):
        g0 = g * GRP
        psbanks[0] = [0, 1, 2, 3, 4, 5]
        cur[0] = acc = accs[g % 2]
        for dq in range(4):
            P.dma("sp", V(acc.t[:, 2 * dq:2 * dq + 2, :], (f"{acc.name}#{2 * dq}", f"{acc.name}#{2 * dq + 1}")),
                  h1s_d.v(h1s_d.t.rearrange("d p t -> p d t")[:, 2 * dq:2 * dq + 2, g0:g0 + GRP]))
            P.dma("sp", xn2[:, 2 * dq:2 * dq + 2, :], xn2s_d.v(xn2s_d.t.rearrange("d p t -> p d t")[:, 2 * dq:2 * dq + 2, g0:g0 + GRP]))
        for n in range(GRP // 128):
            if n % 4 == 0:
                pz = nps()
            tr(pz[0:32, (n % 4) * 128:(n % 4 + 1) * 128], Gtok[:, g * (GRP // 128) + n, :], ident)
            if n % 4 == 3:
                cp(GT[:, (n - 3) * 128:(n + 1) * 128], pz[0:32, :])
        pending = None
        for e in range(32):
            b = it % 2
            if it + 1 < NG * 32:
                load_gu(it + 1)
            ts(sel[:], ones_f[0:32, :], cst[0:32, C_ID + e:C_ID + e + 1], None, ALU.mult)
            for tcn in range(GRP // TC2):
                tk = slice(tcn * TC2, (tcn + 1) * TC2)
                hb_ = hT[un % 2]
                emit_gu(b, tk, hb_)
                if pending is not None:
                    emit_d(*pending)
                if tcn == 0 and it + 1 < NG * 32:
                    load_d(it + 1)
                pending = (b, tk, hb_)
                un += 1
                if deferred:
                    k = -(-len(deferred) // max(1, (32 - e) * (GRP // TC2) - tcn - 2)) if e < 31 else len(deferred)
                    replay(deferred[:k]); del deferred[:k]
            it += 1
        emit_d(*pending)
        replay(deferred); del deferred[:]
        P.begin()
        psbanks[0] = [6, 7]
        for tcn in range(GRP // TC2):
            tk = slice(tcn * TC2, (tcn + 1) * TC2)
            tg0 = g0 + tcn * TC2
            act(sq3[:], accall(tk), AF.Square)
            pz = nps()
            for dt in range(8):
                mm(pz[:], ones_b[:], sq3[:, dt, :], start=(dt == 0), stop=(dt == 7))
            rsqrt_from(rs3[:], pz[:], 1.0 / D)
            for dt in range(8):
                stt(xn3[:, dt, :], accv(dt, tk), spc("g3", dt), rs3[:], ALU.mult, ALU.mult)
            P.dma("sp", ptok[:], p_d.v(p_d.t[tg0:tg0 + TC2, :].rearrange("(n p) d -> p n d", p=128)))
            for k2 in range(2):
                pz = nps()
                for n in range(4):
                    tr(pz[:, n * 128:(n + 1) * 128], ptok[:, n, k2 * 128:(k2 + 1) * 128], ident)
                cp(pT[:, k2, :], pz[:])
            for dt in range(8):
                pgt = nps(); ppl = nps()
                for kt in range(8):
                    mm(pgt[:], wpg[:, kt, dt * 128:(dt + 1) * 128], xn3[:, kt, :], start=(kt == 0), stop=(kt == 7))
                for k2 in range(2):
                    mm(ppl[:], wple[:, k2, dt * 128:(dt + 1) * 128], pT[:, k2, :], start=(k2 == 0), stop=(k2 == 1))
                act(sg3[:], pgt[:], AF.Sigmoid)
                tt(sg3[:], sg3[:], ppl[:], ALU.mult)
                tt(accv(dt, tk), accv(dt, tk), sg3[:], ALU.add)
            act(sq3[:], accall(tk), AF.Square)
            pz = nps()
            for dt in range(8):
                mm(pz[:], ones_b[:], sq3[:, dt, :], start=(dt == 0), stop=(dt == 7))
            rsqrt_from(rs3[:], pz[:], 1.0 / D)
            for dt in range(8):
                stt(accv(dt, tk), accv(dt, tk), spc("gf", dt), rs3[:], ALU.mult, ALU.mult)
            for half in range(2):
                for n2 in range(2):
                    n = half * 2 + n2
                    for dq in range(2):
                        pz = nps()
                        for j in range(4):
                            dt = dq * 4 + j
                            tr(pz[:, j * 128:(j + 1) * 128], accv(dt, slice(tcn * TC2 + n * 128, tcn * TC2 + (n + 1) * 128)), ident)
                        cp(otok[:, n2, dq * 512:(dq + 1) * 512], pz[:], eng=("act" if dq else "dve"))
                r0 = tg0 + half * 256
                P.dma("sp", out_d.v(out_d.t[r0:r0 + 256, :].rearrange("(n p) d -> p n d", p=128)), otok[:])
        deferred = P.rec
        P.rec = None
        if g == NG - 1:
            replay(deferred); del deferred[:]
    global _SB_LEFT2
    _SB_LEFT2 = nc.sbuf_bytes_remaining
    P.barrier()
    s2.close()
    es.close()
    return nc


def _host_layout(inp):
    f = lambda k: np.asarray(inp[k], dtype=np.float32)
    sp = np.zeros((128, NSP), np.float32)

    def put(name, arr):
        o, w = SP[name]
        sp[:, o:o + w] = arr

    put("gmix", f("norm_mix")[0].reshape(8, 128).T)
    put("g2", f("norm_ffn")[0].reshape(8, 128).T)
    put("g3", f("norm_ple")[0].reshape(8, 128).T)
    put("gf", f("norm_final").reshape(8, 128).T)
    put("lcw", f("lru_conv_w")[0].reshape(4, 4, 128).transpose(2, 1, 0).reshape(128, 16))
    put("lcb", f("lru_conv_b")[0].reshape(4, 128).T)
    put("ba", f("lru_ba")[0].reshape(4, 128).T)
    put("bi", f("lru_bi")[0].reshape(4, 128).T)
    put("lam", f("lru_lambda")[0].reshape(4, 128).T)
    put("lon", f("lru_out_norm")[0].reshape(4, 128).T)
    put("gcw", f("gdn_conv_w")[0].reshape(4, 12, 128).transpose(2, 1, 0).reshape(128, 48))
    put("gg", f("gdn_out_norm")[0].reshape(128, 1))
    put("alog", np.broadcast_to(f("gdn_a_log")[0][None, :], (128, 4)))
    put("dtb", np.broadcast_to(f("gdn_dt_bias")[0][None, :], (128, 4)))
    put("br", np.broadcast_to(np.concatenate([f("b_router_group")[0], f("b_router_expert")[0]])[None, :], (128, 36)))
    cst = np.zeros((128, NCONST), np.float32)
    eye = np.eye(128, dtype=np.float32)
    j = np.arange(128)[:, None]; i = np.arange(128)[None, :]
    mu = (i >= j).astype(np.float32); msu = (i > j).astype(np.float32)
    cst[:, C_ID:C_ID + 128] = eye
    cst[:, C_MU:C_MU + 512] = np.tile(mu, (1, 4))
    cst[:, C_MSU:C_MSU + 512] = np.tile(msu, (1, 4))
    cst[:, C_ID4:C_ID4 + 512] = np.tile(eye, (1, 4))
    cst[:, C_TRI:C_TRI + 128] = (j <= i).astype(np.float32)
    wab = np.zeros((8, 128, 128), np.float32)
    for w_i, key in enumerate(("lru_wa", "lru_wi")):
        w = f(key)[0]
        for ct in range(4):
            wab[w_i * 4 + ct, 0:64, 0:64] = w[2 * ct]
            wab[w_i * 4 + ct, 64:128, 64:128] = w[2 * ct + 1]
    shared = {
        "sp": sp, "consts": cst, "w_in": np.ascontiguousarray(f("w_in")[0]), "wab": wab,
        "w_out": np.ascontiguousarray(f("w_out")[0]),
        "wr": np.ascontiguousarray(np.concatenate([f("w_router_group")[0], f("w_router_expert")[0]], axis=1)),
        "wg": np.ascontiguousarray(f("w_exp_gate")[0]), "wu": np.ascontiguousarray(f("w_exp_up")[0]),
        "wd": np.ascontiguousarray(f("w_exp_down")[0]),
        "w_pg": np.ascontiguousarray(f("w_ple_gate")[0]), "w_ple": np.ascontiguousarray(f("w_ple")[0]),
    }
    return shared


def kernel(**inputs):
    shared = _host_layout(inputs)
    x = np.asarray(inputs["x"], dtype=np.float32)
    p = np.asarray(inputs["p"], dtype=np.float32)[0]
    nc = build()
    in_maps = []
    for c in range(NCORES):
        m = dict(shared)
        m["x"] = np.ascontiguousarray(x[c])
        m["p"] = np.ascontiguousarray(p[c])
        in_maps.append(m)
    res = run_bass_kernel_spmd(nc, in_maps, core_ids=list(range(NCORES)))
    out = np.stack([np.asarray(r["out"], dtype=np.float32) for r in res.results], axis=0)
    return out.astype(inputs["x"].dtype, copy=False)
```

```python
import contextlib
import numpy as np
import concourse.bass as bass
import concourse.mybir as mybir
from concourse.bass_utils import run_bass_kernel_spmd

F32 = mybir.dt.float32
BF16 = mybir.dt.bfloat16
AF = mybir.ActivationFunctionType
ALU = mybir.AluOpType
AX = mybir.AxisListType

S = 4096
D = 1024
NCORES = 8
TC = 256
NSUB = TC // 128
NCH = S // TC
INC = 3080
EPS = 1e-6
GRP = 1024
TC2 = 512
SKIP = set()
SCHED = True
STOPAT = 99


class _Stop(Exception):
    pass


def _ck(k):
    if STOPAT <= k:
        raise _Stop()

SP = {}
_o = 0
for _n, _w in [("gmix", 8), ("g2", 8), ("g3", 8), ("gf", 8), ("lcw", 16), ("lcb", 4), ("ba", 4), ("bi", 4), ("lam", 4),
               ("lon", 4), ("gcw", 48), ("gg", 1), ("alog", 4), ("dtb", 4), ("br", 36)]:
    SP[_n] = (_o, _w)
    _o += _w
NSP = _o
C_ID, C_MU, C_MSU, C_ID4, C_TRI = 0, 128, 640, 1152, 1664
NCONST = 1792


class V:
    __slots__ = ("ap", "names")

    def __init__(self, ap, names):
        self.ap = ap
        self.names = names


class Buf:
    def __init__(self, t, name):
        self.t = t
        self.name = name

    def __getitem__(self, idx):
        return V(self.t[idx], (self.name,))

    def v(self, ap):
        return V(ap, (self.name,))


class Tok:
    __slots__ = ("sem", "val", "src")

    def __init__(self, sem, val, src):
        self.sem = sem
        self.val = val
        self.src = src


class Prog:
    def __init__(self, nc, ndsem=8):
        self.nc = nc
        self.eng = {"pe": nc.tensor, "dve": nc.vector, "act": nc.scalar, "pool": nc.gpsimd, "sp": nc.sync}
        self.sem = {k: nc.alloc_semaphore(f"s_{k}") for k in self.eng}
        self.cnt = {k: 0 for k in self.eng}
        self.dsem = {q: [nc.alloc_semaphore(f"d_{q}{i}") for i in range(ndsem)] for q in ("sp", "pool")}
        self.dcnt = {q: [0] * ndsem for q in self.dsem}
        self.drr = {q: 0 for q in self.dsem}
        self.waited = {k: {} for k in self.eng}
        self.lastw = {}
        self.readers = {}
        self.ninst = 0
        self.pend = None

    def _flush(self):
        if self.pend is not None:
            E, ins, tok = self.pend
            self.cnt[E] += 1
            tok.val = self.cnt[E]
            ins.then_inc(self.sem[E], 1)
            self.pend = None

    def _collect(self, E, reads, writes):
        toks = []
        for b in reads:
            t = self.lastw.get(b)
            if t is not None:
                toks.append(t)
        for b in writes:
            t = self.lastw.get(b)
            if t is not None:
                toks.append(t)
            toks.extend(self.readers.get(b, {}).values())
        return [t for t in toks if not (t.src == "pe" and E == "pe")]

    def _wait(self, E, toks):
        needs = {}
        for t in toks:
            key = id(t.sem)
            if self.waited[E].get(key, 0) >= t.val:
                continue
            if needs.get(key, (None, 0))[1] < t.val:
                needs[key] = (t.sem, t.val)
        for key, (sem, val) in needs.items():
            self.eng[E].wait_ge(sem, val)
            self.waited[E][key] = val
            self.ninst += 1

    def _commit(self, E, tok, reads, writes):
        for b in reads:
            self.readers.setdefault(b, {})[(E, id(tok.sem))] = tok
        for b in writes:
            self.lastw[b] = tok
            self.readers[b] = {}

    def begin(self):
        self.rec = []

    def schedule(self):
        rec, self.rec = self.rec, None
        n = len(rec)
        lastw, readers = {}, {}
        preds = [set() for _ in range(n)]
        for i, (kind, E, fn, reads, writes, cost) in enumerate(rec):
            psr = tuple(b for b in reads if b.startswith("ps"))
            rd = tuple(b for b in reads if not b.startswith("ps"))
            wr = tuple(writes) + psr
            for b in rd:
                if b in lastw:
                    preds[i].add(lastw[b])
            for b in wr:
                if b in lastw:
                    preds[i].add(lastw[b])
                preds[i].update(readers.get(b, ()))
            for b in rd:
                readers.setdefault(b, []).append(i)
            for b in wr:
                lastw[b] = i
                readers[b] = []
        succs = [[] for _ in range(n)]
        indeg = [0] * n
        for i in range(n):
            preds[i].discard(i)
            indeg[i] = len(preds[i])
            for p in preds[i]:
                succs[p].append(i)
        rt = [0.0] * n
        free = {}
        bl = [0.0] * n
        for i in range(n - 1, -1, -1):
            m = 0.0
            for sx in succs[i]:
                if bl[sx] > m:
                    m = bl[sx]
            bl[i] = rec[i][5] + 0.1 + m
        ready = [i for i in range(n) if indeg[i] == 0]
        while ready:
            best, bkey = None, None
            for i in ready:
                E = rec[i][1]
                key = (round(max(free.get(E, 0.0), rt[i]), 2), -bl[i], i)
                if bkey is None or key < bkey:
                    best, bkey = i, key
            ready.remove(best)
            kind, E, fn, reads, writes, cost = rec[best]
            start = bkey[0]
            if kind == "dma":
                free[E] = start + 0.15
                self.dma(E, fn[0], fn[1])
            else:
                free[E] = start + cost
                self.op(E, fn, reads, writes)
            fin = start + cost
            for sx in succs[best]:
                lat = 0.06 if rec[sx][1] == E else 0.12
                if rt[sx] < fin + lat:
                    rt[sx] = fin + lat
                indeg[sx] -= 1
                if indeg[sx] == 0:
                    ready.append(sx)

    def op(self, E, emit, reads=(), writes=(), cost=0.3):
        if getattr(self, "rec", None) is not None:
            self.rec.append(("op", E, emit, tuple(reads), tuple(writes), cost))
            return
        psr = tuple(b for b in reads if b.startswith("ps"))
        if psr:
            reads = tuple(b for b in reads if not b.startswith("ps"))
            writes = tuple(writes) + psr
        toks = self._collect(E, reads, writes)
        tok = None
        if self.pend is not None:
            if self.pend[0] == E and not any(t is self.pend[2] for t in toks):
                tok = self.pend[2]
                self.pend = None
            else:
                self._flush()
        self._wait(E, toks)
        ins = emit(self.eng[E])
        if tok is None:
            tok = Tok(self.sem[E], None, E)
        self.pend = (E, ins, tok)
        self._commit(E, tok, reads, writes)
        self.ninst += 1

    def dma(self, Q, out, in_):
        if getattr(self, "rec", None) is not None:
            nbytes = 4.0
            for d in out.ap.shape:
                nbytes *= d
            self.rec.append(("dma", Q, (out, in_), tuple(in_.names), tuple(out.names), 2.0 + nbytes / 150e3))
            return
        self._flush()
        i = self.drr[Q]
        self.drr[Q] = (i + 1) % len(self.dsem[Q])
        sem = self.dsem[Q][i]
        prev = self.dcnt[Q][i]
        key = id(sem)
        if prev and self.waited[Q].get(key, 0) < prev:
            self.eng[Q].wait_ge(sem, prev)
            self.waited[Q][key] = prev
        self._wait(Q, self._collect(Q, in_.names, out.names))
        ins = self.eng[Q].dma_start(out=out.ap, in_=in_.ap)
        self.dcnt[Q][i] += 16
        ins.then_inc(sem, 16)
        tok = Tok(sem, self.dcnt[Q][i], "dma")
        self._commit(Q, tok, in_.names, out.names)
        self.ninst += 1

    def barrier(self):
        self._flush()
        toks = [(self.sem[k], self.cnt[k]) for k in self.eng if self.cnt[k]]
        for q in self.dsem:
            toks += [(s, c) for s, c in zip(self.dsem[q], self.dcnt[q]) if c]
        for E in self.eng:
            for sem, val in toks:
                if self.waited[E].get(id(sem), 0) < val:
                    self.eng[E].wait_ge(sem, val)
                    self.waited[E][id(sem)] = val


def _ap(x):
    return x.ap if isinstance(x, V) else x


def _nm(*xs):
    r = ()
    for x in xs:
        if isinstance(x, V):
            r += x.names
    return r


def build(dbg=0):
    nc = bass.Bass("TRN2", target_bir_lowering=False)
    P = Prog(nc)
    global _NC, _P
    _NC, _P = nc, P

    def din(name, shape, dt=F32):
        return Buf(nc.dram_tensor(name, shape, dt, kind="ExternalInput").ap(), "dram_" + name)

    x_d = din("x", [S, D]); p_d = din("p", [S, 256]); sp_d = din("sp", [128, NSP]); cst_d = din("consts", [128, NCONST])
    win_d = din("w_in", [D, INC]); wab_d = din("wab", [8, 128, 128]); wout_d = din("w_out", [D, D]); wr_d = din("wr", [D, 36])
    wg_d = din("wg", [32, D, 512]); wu_d = din("wu", [32, D, 512]); wd_d = din("wd", [32, 512, D])
    wpg_d = din("w_pg", [D, D]); wple_d = din("w_ple", [256, D])
    out_d = Buf(nc.dram_tensor("out", [S, D], F32, kind="ExternalOutput").ap(), "dram_out")
    skind = "ExternalOutput" if dbg else "Internal"
    h1s_d = Buf(nc.dram_tensor("h1s", [8, 128, S], F32, kind=skind).ap(), "dram_h1s")
    xn2s_d = Buf(nc.dram_tensor("xn2s", [8, 128, S], BF16, kind="Internal").ap(), "dram_xn2s")
    if dbg:
        ymix_d = Buf(nc.dram_tensor("dbg_ymix", [8, 128, S], F32, kind="ExternalOutput").ap(), "dram_ymix")
        g_d = Buf(nc.dram_tensor("dbg_g", [128, 32, 32], F32, kind="ExternalOutput").ap(), "dram_g")

    es = contextlib.ExitStack()
    nb = [0]

    sbc = {}

    def sb(stack, name, shape, dt=F32):
        key = (name, tuple(shape), str(dt))
        if sbc.get("on") and key in sbc:
            return sbc[key]
        if sbc.get("on"):
            sbc[key] = sb_(stack, name, shape, dt)
            return sbc[key]
        return sb_(stack, name, shape, dt)

    def sb_(stack, name, shape, dt=F32):
        nb[0] += 1
        t = stack.enter_context(nc.sbuf_tensor(f"{name}_{nb[0]}", shape, dt))
        return Buf(t, f"{name}_{nb[0]}")

    ps = [Buf(es.enter_context(nc.psum_tensor(f"ps{i}", [128, 512], F32)), f"ps{i}") for i in range(8)]
    psi = [0]

    psbanks = [list(range(8))]

    def nps():
        psi[0] = (psi[0] + 1) % len(psbanks[0])
        return ps[psbanks[0][psi[0]]]

    def fsz(v):
        n = 1
        for d in v.ap.shape[1:]:
            n *= d
        return n

    def mm(out, lhsT, rhs, start=True, stop=True):
        c = 0.03 + fsz(out) / 2400.0 * (4 if lhsT.ap.dtype == F32 else 1)
        P.op("pe", lambda e: e.matmul(out.ap, lhsT.ap, rhs.ap, start=start, stop=stop),
             reads=_nm(lhsT, rhs), writes=out.names, cost=c)

    def tr(out, in_, ident):
        P.op("pe", lambda e: e.transpose(out.ap, in_.ap, ident.ap), reads=_nm(in_, ident), writes=out.names,
             cost=0.03 + fsz(out) / 600.0)

    def act(out, in_, func, scale=1.0, bias=0.0, accum=None):
        kw = {}
        if accum is not None:
            kw["accum_out"] = accum.ap
        P.op("act", lambda e: e.activation(out.ap, in_.ap, func, bias=_ap(bias), scale=_ap(scale), **kw),
             reads=_nm(in_, scale, bias), writes=_nm(out, accum), cost=0.25 + fsz(out) / 1200.0)

    def tt(out, a, b, op, eng="dve"):
        P.op(eng, lambda e: e.tensor_tensor(out.ap, a.ap, b.ap, op), reads=_nm(a, b), writes=out.names,
             cost=(0.12 + fsz(out) / 960.0) * (2.2 if eng == "pool" else 1.0))

    def ts(out, a, s1, s2, op0, op1=None, eng="dve"):
        c = (0.12 + fsz(out) / 960.0) * (2.2 if eng == "pool" else 1.0)
        if op1 is None:
            P.op(eng, lambda e: e.tensor_scalar(out.ap, a.ap, _ap(s1), None, op0), reads=_nm(a, s1), writes=out.names, cost=c)
        else:
            P.op(eng, lambda e: e.tensor_scalar(out.ap, a.ap, _ap(s1), _ap(s2), op0, op1), reads=_nm(a, s1, s2), writes=out.names, cost=c)

    def stt(out, a, s, b, op0, op1, eng="dve"):
        P.op(eng, lambda e: e.scalar_tensor_tensor(out.ap, a.ap, _ap(s), b.ap, op0, op1), reads=_nm(a, s, b), writes=out.names,
             cost=(0.12 + fsz(out) / 960.0) * (2.2 if eng == "pool" else 1.0))

    def cp(out, in_, eng="act"):
        if eng == "act":
            P.op("act", lambda e: e.activation(out.ap, in_.ap, AF.Copy), reads=in_.names, writes=out.names,
                 cost=0.25 + fsz(out) / 1200.0)
        else:
            P.op(eng, lambda e: e.tensor_copy(out.ap, in_.ap), reads=in_.names, writes=out.names,
                 cost=(0.12 + fsz(out) / 960.0) * (2.2 if eng == "pool" else 1.0))

    def recip(out, in_):
        P.op("dve", lambda e: e.reciprocal(out.ap, in_.ap), reads=in_.names, writes=out.names, cost=0.12 + fsz(out) / 960.0)

    def memset(out, val, eng="pool"):
        P.op(eng, lambda e: e.memset(out.ap, val), writes=out.names)

    def rsqrt_from(out, in_ps, scale):
        act(out, in_ps, AF.Ln, scale=scale, bias=EPS)
        act(out, out, AF.Exp, scale=-0.5)

    def sigm(out, in_, scale=1.0, nbias=0.0):
        act(out, in_, AF.Exp, scale=-scale, bias=nbias)
        act(out, out, AF.Ln, bias=1.0)
        act(out, out, AF.Exp, scale=-1.0)

    cst = sb(es, "cst", [128, NCONST]); spt = sb(es, "sp", [128, NSP])
    P.dma("sp", cst[:], cst_d[:]); P.dma("sp", spt[:], sp_d[:])
    ident = cst[:, C_ID:C_ID + 128]
    maskU4 = cst[:, C_MU:C_MU + 512]; maskSU4 = cst[:, C_MSU:C_MSU + 512]; ident4 = cst[:, C_ID4:C_ID4 + 512]
    triU = cst[:, C_TRI:C_TRI + 128]

    def spc(name, i=0, n=1):
        o, w = SP[name]
        return spt[:, o + i:o + i + n]

    ones_f = sb(es, "ones_f", [128, 128]); ones_b = sb(es, "ones_b", [128, 128], BF16)
    memset(ones_f[:], 1.0); memset(ones_b[:], 1.0)
    Gtok = sb(es, "Gtok", [128, 32, 32])
    der = sb(es, "der", [128, 24])
    lam = spc("lam", 0, 4)
    act(der[:, 12:16], lam, AF.Exp, scale=-1.0)
    act(der[:, 12:16], der[:, 12:16], AF.Ln, bias=1.0)
    ts(der[:, 0:4], der[:, 12:16], -8.0, None, ALU.mult)
    ts(der[:, 4:8], der[:, 12:16], -16.0, None, ALU.mult)
    act(der[:, 12:16], spc("alog", 0, 4), AF.Exp)
    ts(der[:, 8:12], der[:, 12:16], -1.0, None, ALU.mult)
    negA = der[:, 8:12]
    ts(der[:, 16:20], spc("ba", 0, 4), -1.0, None, ALU.mult)
    ts(der[:, 20:24], spc("bi", 0, 4), -1.0, None, ALU.mult)

    s1 = contextlib.ExitStack()
    wba = sb(s1, "wba", [128, 8, 8], BF16)
    P.dma("pool", wba[:], win_d.v(win_d.t[:, 3072:3080].rearrange("(kt p) c -> p kt c", p=128)))
    wst = [sb(s1, f"wst{i}", [128, 8, 256], BF16) for i in range(3)]
    wrr = [0]
    wab = sb(s1, "wab", [128, 8, 128], BF16)
    P.dma("pool", wab[:], wab_d.v(wab_d.t.rearrange("n p c -> p n c")))
    wr = sb(s1, "wr", [128, 8, 36])
    P.dma("sp", wr[:], wr_d.v(wr_d.t.rearrange("(kt p) c -> p kt c", p=128)))
    wout = sb(s1, "wout", [128, 8, D], BF16)
    for q in range(4):
        P.dma("pool", wout[:, 2 * q:2 * q + 2, :], wout_d.v(wout_d.t[256 * q:256 * (q + 1), :].rearrange("(kt p) c -> p kt c", p=128)))
    NT = TC // 128
    xtokP = [sb(s1, f"xtok{i}", [128, NT, D]) for i in range(2)]
    xTP = [sb(s1, f"xT{i}", [128, 8, TC]) for i in range(2)]
    sqP = [sb(s1, f"sq{i}", [128, 8, TC], BF16) for i in range(2)]
    xnTP = [sb(s1, f"xnT{i}", [128, 8, TC], BF16) for i in range(2)]
    rstdP = [sb(s1, f"rstd{i}", [128, TC]) for i in range(2)]
    ymixP = [sb(s1, f"ymix{i}", [128, 8, TC], BF16) for i in range(2)]
    halo = sb(s1, "halo", [128, 16, 4]); memset(halo[:], 0.0)
    hst = sb(s1, "hst", [128, 4]); memset(hst[:], 0.0)
    Sst = sb(s1, "Sst", [128, 512]); memset(Sst[:], 0.0)
    Sb = sb(s1, "Sb", [128, 512], BF16); memset(Sb[:], 0.0)
    xn2b = sb(s1, "xn2b", [128, 8, TC], BF16)

    def conv(pz, hidx, cb, xc, wname, widx, bias=None, eng="dve"):
        cp(cb[:, 0:3], halo[:, hidx, 0:3], eng=eng)
        cp(cb[:, 3:TC + 3], pz[:, 0:TC], eng="dve")
        cp(halo[:, hidx, 0:3], cb[:, TC:TC + 3], eng=eng)
        w = lambda j: spc(wname, widx * 4 + j)
        if bias is None:
            ts(xc, cb[:, 0:TC], w(0), None, ALU.mult, eng=eng)
        else:
            ts(xc, cb[:, 0:TC], w(0), bias, ALU.mult, ALU.add, eng=eng)
        for j in range(1, 4):
            stt(xc, cb[:, j:j + TC], w(j), xc, ALU.mult, ALU.add, eng=eng)

    sbc["on"] = True

    def body(c):
        t0 = c * TC
        par = c % 2
        psbanks[0] = [0, 1, 2]
        xtok = xtokP[par]; xT = xTP[par]; sq = sqP[par]; xnT = xnTP[par]; rstd = rstdP[par]; ymix = ymixP[par]
        wcache = {}

        def proj(col0, width=128):
            grp = col0 // 256
            if grp not in wcache:
                wb = wst[wrr[0] % 3]
                wrr[0] += 1
                P.dma("pool", wb[:], win_d.v(win_d.t[:, 256 * grp:256 * (grp + 1)].rearrange("(kt p) c -> p kt c", p=128)))
                wcache[grp] = wb
            wb = wcache[grp]
            off = col0 - 256 * grp
            pz = nps()
            for kt in range(8):
                mm(pz[:width, 0:TC], wb[:, kt, off:off + width], xnT[:, kt, :], start=(kt == 0), stop=(kt == 7))
            return pz

        P.dma("sp", xtok[:], x_d.v(x_d.t[t0:t0 + TC, :].rearrange("(n p) d -> p n d", p=128)))
        for dt in range(8):
            pz = nps()
            for n in range(NT):
                tr(pz[:, n * 128:(n + 1) * 128], xtok[:, n, dt * 128:(dt + 1) * 128], ident)
            cp(xT[:, dt, :], pz[:, 0:TC], eng=("act" if dt % 2 else "dve"))
        act(sq[:], xT[:], AF.Square)
        pz = nps()
        for dt in range(8):
            mm(pz[:, 0:TC], ones_b[:], sq[:, dt, :], start=(dt == 0), stop=(dt == 7))
        rsqrt_from(rstd[:], pz[:, 0:TC], 1.0 / D)
        for dt in range(8):
            stt(xnT[:, dt, :], xT[:, dt, :], spc("gmix", dt), rstd[:], ALU.mult, ALU.mult)

        for sl in (s1,):
          if 'lru' not in SKIP:
                T = lambda n, dt=F32, w=TC: sb(sl, "L" + n, [128, w], dt)
                cbL = [T("cb0", F32, TC + 3), T("cb1", F32, TC + 3)]; xcL = [T("xc0"), T("xc1")]; xcbL = [T("xcb0", BF16), T("xcb1", BF16)]
                r = T("r"); ig = T("ig"); a = T("a"); s_ = T("s")
                hb = T("hb"); gb = T("gb"); tmp = T("tmp")
                yl = sb(sl, "yl", [128, 4, TC]); ysq = sb(sl, "ysq", [128, 4, TC], BF16); rs = T("rs")
                for ct in range(4):
                    cb = cbL[ct % 2]; xc = xcL[ct % 2]; xcb = xcbL[ct % 2]
                    pz = proj(ct * 128)
                    conv(pz, ct, cb, xc[:], "lcw", ct, bias=spc("lcb", ct))
                    cp(xcb[:], xc[:])
                    pr = nps(); mm(pr[:, 0:TC], wab[:, ct, :], xcb[:])
                    sigm(r[:], pr[:, 0:TC], nbias=der[:, 16 + ct:17 + ct])
                    pi = nps(); mm(pi[:, 0:TC], wab[:, 4 + ct, :], xcb[:])
                    sigm(ig[:], pi[:, 0:TC], nbias=der[:, 20 + ct:21 + ct])
                    act(a[:], r[:], AF.Exp, scale=der[:, ct:ct + 1])
                    act(s_[:], r[:], AF.Exp, scale=der[:, 4 + ct:5 + ct])
                    act(s_[:], s_[:], AF.Ln, scale=-1.0, bias=1.0)
                    act(s_[:], s_[:], AF.Exp, scale=0.5)
                    tt(s_[:], s_[:], ig[:], ALU.mult, eng="pool")
                    tt(s_[:], s_[:], xc[:], ALU.mult)
                    P.op("dve", lambda e, ct=ct: e.tensor_tensor_scan(hb.t[:], a.t[:], s_.t[:], hst.t[:, ct:ct + 1], ALU.mult, ALU.add),
                         reads=(a.name, s_.name, hst.name), writes=(hb.name,))
                    cp(hst[:, ct:ct + 1], hb[:, TC - 1:TC], eng="dve")
                    pg = proj(512 + ct * 128)
                    cp(gb[:], pg[:, 0:TC])
                    tt(tmp[:], gb[:], gb[:], ALU.mult, eng="pool")
                    ts(tmp[:], tmp[:], 0.044715, 1.0, ALU.mult, ALU.add)
                    tt(tmp[:], tmp[:], gb[:], ALU.mult)
                    sigm(tmp[:], tmp[:], scale=1.5957691216057308)
                    tt(tmp[:], tmp[:], gb[:], ALU.mult)
                    tt(yl[:, ct, :], hb[:], tmp[:], ALU.mult)
                    act(ysq[:, ct, :], yl[:, ct, :], AF.Square)
                pz = nps()
                for ct in range(4):
                    mm(pz[:, 0:TC], ones_b[:], ysq[:, ct, :], start=(ct == 0), stop=(ct == 3))
                rsqrt_from(rs[:], pz[:, 0:TC], 1.0 / 512)
                for ct in range(4):
                    stt(ymix[:, ct, :], yl[:, ct, :], spc("lon", ct), rs[:], ALU.mult, ALU.mult)

        for sg in (s1,):
          if 'gdn' not in SKIP:
                T = lambda n, dt=F32, w=512: sb(sg, n, [128, w], dt)
                cbs = [T(f"cb{i}", F32, TC + 3) for i in range(3)]; xcs = [T(f"xc{i}", F32, TC) for i in range(3)]
                qn = sb(sg, f"qn{par}", [128, 4, TC]); kn = sb(sg, f"kn{par}", [128, 4, TC]); vT = sb(sg, f"vT{par}", [128, 4, TC])
                zs = sb(sg, f"zs{par}", [128, 4, TC], BF16)
                rns = [T("rn0", F32, TC), T("rn1", F32, TC)]
                for tile in range(12):
                    pz = proj(1024 + tile * 128)
                    cb = cbs[tile % 3]; xc = xcs[tile % 3]
                    conv(pz, 4 + tile, cb, xc[:], "gcw", tile)
                    dst = (qn, kn, vT)[tile // 4]
                    sigm(dst[:, tile % 4, :], xc[:])
                    tt(dst[:, tile % 4, :], dst[:, tile % 4, :], xc[:], ALU.mult, eng="pool")
                for h in range(4):
                    pz = proj(2560 + h * 128)
                    rn = rns[h % 2]
                    sigm(rn[:], pz[:, 0:TC])
                    tt(zs[:, h, :], rn[:], pz[:, 0:TC], ALU.mult)
                for which, dst in ((0, qn), (1, kn)):
                    act(sq[:, 0:4, :], dst[:], AF.Square)
                    for h in range(4):
                        pz = nps()
                        rn = rns[h % 2]
                        mm(pz[:, 0:TC], ones_b[:], sq[:, h, :])
                        rsqrt_from(rn[:], pz[:, 0:TC], 1.0)
                        if which == 0:
                            stt(dst[:, h, :], dst[:, h, :], 128.0 ** -0.5, rn[:], ALU.mult, ALU.mult)
                        else:
                            tt(dst[:, h, :], dst[:, h, :], rn[:], ALU.mult)
                yield
                psbanks[0] = [3, 4, 5, 6, 7]
                bal = sb(sg, "bal", [128, 8]); beta = sb(sg, "beta", [128, 4]); g_ = sb(sg, "g", [128, 4]); gcl = sb(sg, "gcl", [128, 8])
                sc8 = sb(sg, "sc8", [128, 24])
                dg = sb(sg, "dg", [128, 8, 128]); egcR = T("egcR"); arg = T("arg"); dT = T("dT"); Dm = T("Dm"); DmS = T("DmS")
                NB = T("NB"); Pk = [T("P0"), T("P1")]; Lk = [T("L0"), T("L1")]; X = T("X"); QK = T("QK", BF16); ATb = T("ATb", BF16)
                Kbg = T("Kbg", BF16); kdec = T("kdec", BF16); Vb = T("Vb", BF16); U = T("U"); WT = T("WT", BF16); qdT = T("qdT", BF16)
                vnew = T("vnew", BF16); o_ = T("o"); osq = T("osq", BF16); rs = T("rs")
                H = lambda h: slice(h * 128, (h + 1) * 128)
                for sc in range(NSUB if 'gdn_sub' not in SKIP else 0):
                    tk = slice(sc * 128, (sc + 1) * 128)
                    pz = nps()
                    for kt in range(8):
                        mm(pz[:, 0:8], xnT[:, kt, tk], wba[:, kt, :], start=(kt == 0), stop=(kt == 7))
                    cp(bal[:], pz[:, 0:8])
                    sigm(beta[:], bal[:, 0:4])
                    tt(g_[:], bal[:, 4:8], spc("dtb", 0, 4), ALU.add)
                    act(g_[:], g_[:], AF.Exp)
                    act(g_[:], g_[:], AF.Ln, bias=1.0)
                    tt(g_[:], g_[:], negA, ALU.mult)
                    _ck(1)
                    pz = nps()
                    mm(pz[:, 0:4], triU, g_[:]); mm(pz[:, 4:8], ones_f[:], g_[:])
                    cp(gcl[:], pz[:, 0:8])
                    gc = lambda h: gcl[:, h:h + 1]
                    ts(sc8[:, 0:4], gcl[:, 0:4], -1.0, None, ALU.mult)
                    act(sc8[:, 4:8], gcl[:, 0:4], AF.Exp)
                    tt(sc8[:, 8:12], gcl[:, 4:8], gcl[:, 0:4], ALU.subtract)
                    act(sc8[:, 8:12], sc8[:, 8:12], AF.Exp)
                    tt(sc8[:, 12:16], beta[:], sc8[:, 4:8], ALU.mult)
                    act(sc8[:, 16:20], gcl[:, 4:8], AF.Exp)
                    _ck(2)
                    for h in range(4):
                        ts(dg[:, h, :], ident, gcl[:, h:h + 1], None, ALU.mult)
                        ts(dg[:, 4 + h, :], ident, beta[:, h:h + 1], None, ALU.mult)
                    pR1 = nps(); mm(pR1[:], ones_f[:], dg[:, 0:4, :])
                    pR2 = nps(); mm(pR2[:], ones_f[:], dg[:, 4:8, :])
                    _ck(3)
                    act(egcR[:], pR1[:], AF.Exp)
                    for h in range(4):
                        ts(arg[:, H(h)], pR1[:, H(h)], sc8[:, h:h + 1], 0.0, ALU.add, ALU.min)
                    act(dT[:], arg[:], AF.Exp)
                    tt(Dm[:], dT[:], maskU4, ALU.mult, eng="pool")
                    tt(DmS[:], dT[:], maskSU4, ALU.mult, eng="pool")
                    tt(NB[:], DmS[:], pR2[:], ALU.mult)
                    _ck(4)
                    pK = nps(); pQ = nps()
                    for h in range(4):
                        mm(pK[:, H(h)], kn[:, h, tk], kn[:, h, tk])
                    for h in range(4):
                        mm(pQ[:, H(h)], kn[:, h, tk], qn[:, h, tk])
                    tt(Pk[0][:], pK[:], NB[:], ALU.mult)
                    tt(QK[:], pQ[:], Dm[:], ALU.mult)
                    _ck(5)
                    pT = nps()
                    for h in range(4):
                        tr(pT[:, H(h)], Pk[0][:, H(h)], ident)
                    cp(Lk[0][:], pT[:])
                    tt(X[:], ident4, Pk[0][:], ALU.subtract)
                    _ck(6)
                    cur = 0
                    for lev in range(1, 7):
                        nxt = 1 - cur
                        pL = nps()
                        if lev < 6:
                            pP = nps()
                            for h in range(4):
                                mm(pP[:, H(h)], Lk[cur][:, H(h)], Pk[cur][:, H(h)])
                            cp(Pk[nxt][:], pP[:])
                            for h in range(4):
                                tr(pL[:, H(h)], Pk[nxt][:, H(h)], ident)
                        else:
                            for h in range(4):
                                mm(pL[:, H(h)], Pk[cur][:, H(h)], Lk[cur][:, H(h)])
                        cp(Lk[nxt][:], pL[:])
                        pX = nps()
                        for h in range(4):
                            mm(pX[:, H(h)], Lk[nxt][:, H(h)], X[:, H(h)])
                        tt(X[:], X[:], pX[:], ALU.add)
                        cur = nxt
                    cp(ATb[:], X[:])
                    _ck(7)
                    pKt = nps(); pVt = nps()
                    for h in range(4):
                        tr(pKt[:, H(h)], kn[:, h, tk], ident)
                    for h in range(4):
                        tr(pVt[:, H(h)], vT[:, h, tk], ident)
                    for h in range(4):
                        ts(Kbg[:, H(h)], pKt[:, H(h)], sc8[:, 12 + h:13 + h], None, ALU.mult)
                        ts(kdec[:, H(h)], pKt[:, H(h)], sc8[:, 8 + h:9 + h], None, ALU.mult)
                        ts(Vb[:, H(h)], pVt[:, H(h)], beta[:, h:h + 1], None, ALU.mult)
                    _ck(8)
                    pU = nps(); pW = nps()
                    for h in range(4):
                        mm(pU[:, H(h)], ATb[:, H(h)], Vb[:, H(h)])
                    for h in range(4):
                        mm(pW[:, H(h)], Kbg[:, H(h)], ATb[:, H(h)])
                    cp(U[:], pU[:])
                    cp(WT[:], pW[:], eng="dve")
                    _ck(9)
                    tt(qdT.v(qdT.t[:].rearrange("p (h c) -> p h c", h=4)), qn[:, :, tk],
                       egcR.v(egcR.t[:].rearrange("p (h c) -> p h c", h=4)), ALU.mult)
                    pWS = nps()
                    for h in range(4):
                        mm(pWS[:, H(h)], WT[:, H(h)], Sb[:, H(h)])
                    tt(vnew[:], U[:], pWS[:], ALU.subtract)
                    pO = nps()
                    for h in range(4):
                        mm(pO[:, H(h)], Sb[:, H(h)], qdT[:, H(h)], start=True, stop=False)
                        mm(pO[:, H(h)], vnew[:, H(h)], QK[:, H(h)], start=False, stop=True)
                    pS = nps()
                    for h in range(4):
                        mm(pS[:, H(h)], kdec[:, H(h)], vnew[:, H(h)])
                    for h in range(4):
                        stt(Sst[:, H(h)], Sst[:, H(h)], sc8[:, 16 + h:17 + h], pS[:, H(h)], ALU.mult, ALU.add)
                    cp(Sb[:], Sst[:])
                    _ck(10)
                    cp(o_[:], pO[:], eng="dve")
                    act(osq[:], pO[:], AF.Square)
                    pN = nps(); mm(pN[:], ones_b[:], osq[:])
                    rsqrt_from(rs[:], pN[:], 1.0 / 128)
                    tt(o_[:], o_[:], rs[:], ALU.mult)
                    stt(ymix[:, 4:8, tk], o_.v(o_.t[:].rearrange("p (h c) -> p h c", h=4)), spc("gg", 0), zs[:, :, tk], ALU.mult, ALU.mult)

        if dbg:
            for kt in range(8):
                P.dma("pool", ymix_d.v(ymix_d.t[kt, :, t0:t0 + TC]), ymix[:, kt, :])
        for dt in range(8):
            pz = nps()
            for kt in range(8):
                mm(pz[:, 0:TC], wout[:, kt, dt * 128:(dt + 1) * 128], ymix[:, kt, :], start=(kt == 0), stop=(kt == 7))
            tt(xT[:, dt, :], xT[:, dt, :], pz[:, 0:TC], ALU.add)
        act(sq[:], xT[:], AF.Square)
        pz = nps()
        for dt in range(8):
            mm(pz[:, 0:TC], ones_b[:], sq[:, dt, :], start=(dt == 0), stop=(dt == 7))
        rsqrt_from(rstd[:], pz[:, 0:TC], 1.0 / D)
        xn2f = xtok.v(xtok.t[:].rearrange("p n d -> p (n d)").rearrange("p (k t) -> p k t", k=8))
        xn2f_k = lambda kt, sl: xtok.v(xtok.t[:].rearrange("p n d -> p (n d)").rearrange("p (k t) -> p k t", k=8)[:, kt, sl])
        for dt in range(8):
            stt(xn2f_k(dt, slice(0, TC)), xT[:, dt, :], spc("g2", dt), rstd[:], ALU.mult, ALU.mult)
        cp(xn2b[:], xn2f)
        for sr in (s1,):
            R = lambda n, w: sb(sr, n, [128, w])
            lg = R("lg", 36); gmax = R("gmax", 2); oh = R("oh", 4); eg = R("eg", 4); sume = R("sume", 2); pen = R("pen", 4)
            ml = R("ml", 32); m8 = R("m8", 8); is1 = R("is1", 32); is2 = R("is2", 32); w12 = R("w12", 4)
            for n in range(NT):
                tkn = slice(n * 128, (n + 1) * 128)
                pz = nps()
                for kt in range(8):
                    mm(pz[:, 0:36], xn2f_k(kt, tkn), wr[:, kt, :], start=(kt == 0), stop=(kt == 7))
                tt(lg[:], pz[:, 0:36], spc("br", 0, 36), ALU.add)
                P.op("dve", lambda e: e.reduce_max(gmax.t[:, 0:1], lg.t[:, 0:4], AX.X), reads=(lg.name,), writes=(gmax.name,))
                ts(oh[:], lg[:, 0:4], gmax[:, 0:1], None, ALU.is_equal)
                ts(gmax[:, 1:2], gmax[:, 0:1], -1.0, None, ALU.mult)
                act(eg[:], lg[:, 0:4], AF.Exp, bias=gmax[:, 1:2], accum=sume[:, 0:1])
                recip(sume[:, 1:2], sume[:, 0:1])
                ts(pen[:], oh[:], 1e30, -1e30, ALU.mult, ALU.add)
                tt(ml.v(ml.t[:].rearrange("p (g j) -> p g j", g=4)), lg.v(lg.t[:, 4:36].rearrange("p (g j) -> p g j", g=4)),
                   pen.v(pen.t[:].unsqueeze(2).to_broadcast([128, 4, 8])), ALU.add)
                P.op("dve", lambda e: e.max(m8.t[:], ml.t[:]), reads=(ml.name,), writes=(m8.name,))
                ts(is1[:], ml[:], m8[:, 0:1], None, ALU.is_equal)
                ts(is2[:], ml[:], m8[:, 1:2], None, ALU.is_equal)
                tt(w12[:, 0:1], m8[:, 1:2], m8[:, 0:1], ALU.subtract)
                act(w12[:, 0:1], w12[:, 0:1], AF.Exp)
                ts(w12[:, 1:2], w12[:, 0:1], 1.0, None, ALU.add)
                recip(w12[:, 1:2], w12[:, 1:2])
                tt(w12[:, 2:3], w12[:, 1:2], sume[:, 1:2], ALU.mult)
                tt(w12[:, 3:4], w12[:, 2:3], w12[:, 0:1], ALU.mult)
                tile_i = c * NT + n
                ts(is1[:], is1[:], w12[:, 2:3], None, ALU.mult)
                stt(Gtok[:, tile_i, :], is2[:], w12[:, 3:4], is1[:], ALU.mult, ALU.add)
        P.dma("sp", h1s_d.v(h1s_d.t.rearrange("d p t -> p d t")[:, :, t0:t0 + TC]), xT[:])
        P.dma("sp", xn2s_d.v(xn2s_d.t.rearrange("d p t -> p d t")[:, :, t0:t0 + TC]), xn2b[:])

    nch = NCH if dbg != 9 else 2
    gens = {0: body(0)}
    P.begin(); next(gens[0]); P.schedule()
    for c in range(nch):
        P.begin()
        for _ in gens.pop(c):
            pass
        if c + 1 < nch:
            gens[c + 1] = body(c + 1)
            next(gens[c + 1])
        P.schedule()
    sbc["on"] = False
    global _SB_LEFT
    _SB_LEFT = nc.sbuf_bytes_remaining
    if dbg:
        P.dma("sp", g_d[:], Gtok[:])
    P.barrier()
    s1.close()

    if dbg in (1, 9):
        P.barrier()
        es.close()
        return nc

    s2 = contextlib.ExitStack()
    accs = [sb(s2, "accA", [128, 8, GRP]), sb(s2, "accB", [128, 8, GRP])]
    cur = [accs[0]]
    xn2 = sb(s2, "xn2", [128, 8, GRP], BF16)
    wgb = [sb(s2, f"wg{i}", [128, 8, 512], BF16) for i in range(2)]
    wub = [sb(s2, f"wu{i}", [128, 8, 512], BF16) for i in range(2)]
    wdb = [sb(s2, f"wd{i}", [128, 4, D], BF16) for i in range(2)]
    GT = sb(s2, "GT", [32, GRP])
    sel = sb(s2, "sel", [32, 128])
    gs = sb(s2, "gs", [128, TC2]); sgT = [sb(s2, f"sg{i}", [128, TC2]) for i in range(2)]
    hT = [sb(s2, f"hT{i}", [128, 4, TC2], BF16) for i in range(2)]
    wpg = sb(s2, "wpg", [128, 8, D], BF16); wple = sb(s2, "wple", [128, 2, D], BF16)
    for q in range(4):
        P.dma("pool", wpg[:, 2 * q:2 * q + 2, :], wpg_d.v(wpg_d.t[256 * q:256 * (q + 1), :].rearrange("(kt p) c -> p kt c", p=128)))
    P.dma("pool", wple[:], wple_d.v(wple_d.t.rearrange("(kt p) c -> p kt c", p=128)))
    rs3 = sb(s2, "rs3", [128, TC2]); sq3 = sb(s2, "sq3", [128, 8, TC2], BF16); xn3 = sq3
    ptok = sb(s2, "ptok", [128, 4, 256]); pT = sb(s2, "pT", [128, 2, TC2], BF16)
    sg3 = sb(s2, "sg3", [128, TC2]); otok = sb(s2, "otok", [128, 2, D])

    def load_gu(i):
        e = i % 32
        b = i % 2
        for q in range(4):
            P.dma("pool", wgb[b][:, 2 * q:2 * q + 2, :], wg_d.v(wg_d.t[e, 256 * q:256 * (q + 1), :].rearrange("(kt p) f -> p kt f", p=128)))
            P.dma("pool", wub[b][:, 2 * q:2 * q + 2, :], wu_d.v(wu_d.t[e, 256 * q:256 * (q + 1), :].rearrange("(kt p) f -> p kt f", p=128)))

    def load_d(i):
        for q in range(2):
            P.dma("pool", wdb[i % 2][:, 2 * q:2 * q + 2, :], wd_d.v(wd_d.t[i % 32, 256 * q:256 * (q + 1), :].rearrange("(ft p) d -> p ft d", p=128)))

    def accv(d, tk):
        return V(cur[0].t[:, d, tk], (f"{cur[0].name}#{d}",))

    def accall(tk):
        return V(cur[0].t[:, :, tk], tuple(f"{cur[0].name}#{d}" for d in range(8)))

    def replay(ops):
        for kind, E, fn, reads, writes, cost in ops:
            if kind == "dma":
                P.dma(E, fn[0], fn[1])
            else:
                P.op(E, fn, reads, writes)

    def emit_gu(b, tk, hb_):
        pgs = nps(); mm(pgs[:], sel[:], GT[:, tk])
        cp(gs[:], pgs[:])
        for f in range(4):
            pg_ = nps(); pu_ = nps()
            for kt in range(8):
                mm(pg_[:], wgb[b][:, kt, f * 128:(f + 1) * 128], xn2[:, kt, tk], start=(kt == 0), stop=(kt == 7))
            for kt in range(8):
                mm(pu_[:], wub[b][:, kt, f * 128:(f + 1) * 128], xn2[:, kt, tk], start=(kt == 0), stop=(kt == 7))
            st = sgT[f % 2]
            act(st[:], pg_[:], AF.Silu)
            tt(st[:], st[:], pu_[:], ALU.mult)
            tt(hb_[:, f, :], st[:], gs[:], ALU.mult)

    def emit_d(b, tk, hb_):
        for d in range(8):
            pd_ = nps()
            for f in range(4):
                mm(pd_[:], wdb[b][:, f, d * 128:(d + 1) * 128], hb_[:, f, :], start=(f == 0), stop=(f == 3))
            tt(accv(d, tk), accv(d, tk), pd_[:], ALU.add)

    NG = S // GRP
    load_gu(0); load_d(0)
    it = 0
    un = 0
    deferred = []
    for g in range(NG):
        g0 = g * GRP
        psbanks[0] = [0, 1, 2, 3, 4, 5]
        cur[0] = acc = accs[g % 2]
        for dq in range(4):
            P.dma("sp", V(acc.t[:, 2 * dq:2 * dq + 2, :], (f"{acc.name}#{2 * dq}", f"{acc.name}#{2 * dq + 1}")),
                  h1s_d.v(h1s_d.t.rearrange("d p t -> p d t")[:, 2 * dq:2 * dq + 2, g0:g0 + GRP]))
            P.dma("sp", xn2[:, 2 * dq:2 * dq + 2, :], xn2s_d.v(xn2s_d.t.rearrange("d p t -> p d t")[:, 2 * dq:2 * dq + 2, g0:g0 + GRP]))
        for n in range(GRP // 128):
            if n % 4 == 0:
                pz = nps()
            tr(pz[0:32, (n % 4) * 128:(n % 4 + 1) * 128], Gtok[:, g * (GRP // 128) + n, :], ident)
            if n % 4 == 3:
                cp(GT[:, (n - 3) * 128:(n + 1) * 128], pz[0:32, :])
        pending = None
        for e in range(32):
            b = it % 2
            if it + 1 < NG * 32:
                load_gu(it + 1)
            ts(sel[:], ones_f[0:32, :], cst[0:32, C_ID + e:C_ID + e + 1], None, ALU.mult)
            for tcn in range(GRP // TC2):
                tk = slice(tcn * TC2, (tcn + 1) * TC2)
                hb_ = hT[un % 2]
                emit_gu(b, tk, hb_)
                if pending is not None:
                    emit_d(*pending)
                if tcn == 0 and it + 1 < NG * 32:
                    load_d(it + 1)
                pending = (b, tk, hb_)
                un += 1
                if deferred:
                    k = -(-len(deferred) // max(1, (32 - e) * (GRP // TC2) - tcn - 2)) if e < 31 else len(deferred)
                    replay(deferred[:k]); del deferred[:k]
            it += 1
        emit_d(*pending)
        replay(deferred); del deferred[:]
        P.begin()
        psbanks[0] = [6, 7]
        for tcn in range(GRP // TC2):
            tk = slice(tcn * TC2, (tcn + 1) * TC2)
            tg0 = g0 + tcn * TC2
            act(sq3[:], accall(tk), AF.Square)
            pz = nps()
            for dt in range(8):
                mm(pz[:], ones_b[:], sq3[:, dt, :], start=(dt == 0), stop=(dt == 7))
            rsqrt_from(rs3[:], pz[:], 1.0 / D)
            for dt in range(8):
                stt(xn3[:, dt, :], accv(dt, tk), spc("g3", dt), rs3[:], ALU.mult, ALU.mult)
            P.dma("sp", ptok[:], p_d.v(p_d.t[tg0:tg0 + TC2, :].rearrange("(n p) d -> p n d", p=128)))
            for k2 in range(2):
                pz = nps()
                for n in range(4):
                    tr(pz[:, n * 128:(n + 1) * 128], ptok[:, n, k2 * 128:(k2 + 1) * 128], ident)
                cp(pT[:, k2, :], pz[:])
            for dt in range(8):
                pgt = nps(); ppl = nps()
                for kt in range(8):
                    mm(pgt[:], wpg[:, kt, dt * 128:(dt + 1) * 128], xn3[:, kt, :], start=(kt == 0), stop=(kt == 7))
                for k2 in range(2):
                    mm(ppl[:], wple[:, k2, dt * 128:(dt + 1) * 128], pT[:, k2, :], start=(k2 == 0), stop=(k2 == 1))
                act(sg3[:], pgt[:], AF.Sigmoid)
                tt(sg3[:], sg3[:], ppl[:], ALU.mult)
                tt(accv(dt, tk), accv(dt, tk), sg3[:], ALU.add)
            act(sq3[:], accall(tk), AF.Square)
            pz = nps()
            for dt in range(8):
                mm(pz[:], ones_b[:], sq3[:, dt, :], start=(dt == 0), stop=(dt == 7))
            rsqrt_from(rs3[:], pz[:], 1.0 / D)
            for dt in range(8):
                stt(accv(dt, tk), accv(dt, tk), spc("gf", dt), rs3[:], ALU.mult, ALU.mult)
            for half in range(2):
                for n2 in range(2):
                    n = half * 2 + n2
                    for dq in range(2):
                        pz = nps()
                        for j in range(4):
                            dt = dq * 4 + j
                            tr(pz[:, j * 128:(j + 1) * 128], accv(dt, slice(tcn * TC2 + n * 128, tcn * TC2 + (n + 1) * 128)), ident)
                        cp(otok[:, n2, dq * 512:(dq + 1) * 512], pz[:], eng=("act" if dq else "dve"))
                r0 = tg0 + half * 256
                P.dma("sp", out_d.v(out_d.t[r0:r0 + 256, :].rearrange("(n p) d -> p n d", p=128)), otok[:])
        deferred = P.rec
        P.rec = None
        if g == NG - 1:
            replay(deferred); del deferred[:]
    global _SB_LEFT2
    _SB_LEFT2 = nc.sbuf_bytes_remaining
    P.barrier()
    s2.close()
    es.close()
    return nc


def _host_layout(inp):
    f = lambda k: np.asarray(inp[k], dtype=np.float32)
    sp = np.zeros((128, NSP), np.float32)

    def put(name, arr):
        o, w = SP[name]
        sp[:, o:o + w] = arr

    put("gmix", f("norm_mix")[0].reshape(8, 128).T)
    put("g2", f("norm_ffn")[0].reshape(8, 128).T)
    put("g3", f("norm_ple")[0].reshape(8, 128).T)
    put("gf", f("norm_final").reshape(8, 128).T)
    put("lcw", f("lru_conv_w")[0].reshape(4, 4, 128).transpose(2, 1, 0).reshape(128, 16))
    put("lcb", f("lru_conv_b")[0].reshape(4, 128).T)
    put("ba", f("lru_ba")[0].reshape(4, 128).T)
    put("bi", f("lru_bi")[0].reshape(4, 128).T)
    put("lam", f("lru_lambda")[0].reshape(4, 128).T)
    put("lon", f("lru_out_norm")[0].reshape(4, 128).T)
    put("gcw", f("gdn_conv_w")[0].reshape(4, 12, 128).transpose(2, 1, 0).reshape(128, 48))
    put("gg", f("gdn_out_norm")[0].reshape(128, 1))
    put("alog", np.broadcast_to(f("gdn_a_log")[0][None, :], (128, 4)))
    put("dtb", np.broadcast_to(f("gdn_dt_bias")[0][None, :], (128, 4)))
    put("br", np.broadcast_to(np.concatenate([f("b_router_group")[0], f("b_router_expert")[0]])[None, :], (128, 36)))
    cst = np.zeros((128, NCONST), np.float32)
    eye = np.eye(128, dtype=np.float32)
    j = np.arange(128)[:, None]; i = np.arange(128)[None, :]
    mu = (i >= j).astype(np.float32); msu = (i > j).astype(np.float32)
    cst[:, C_ID:C_ID + 128] = eye
    cst[:, C_MU:C_MU + 512] = np.tile(mu, (1, 4))
    cst[:, C_MSU:C_MSU + 512] = np.tile(msu, (1, 4))
    cst[:, C_ID4:C_ID4 + 512] = np.tile(eye, (1, 4))
    cst[:, C_TRI:C_TRI + 128] = (j <= i).astype(np.float32)
    wab = np.zeros((8, 128, 128), np.float32)
    for w_i, key in enumerate(("lru_wa", "lru_wi")):
        w = f(key)[0]
        for ct in range(4):
            wab[w_i * 4 + ct, 0:64, 0:64] = w[2 * ct]
            wab[w_i * 4 + ct, 64:128, 64:128] = w[2 * ct + 1]
    shared = {
        "sp": sp, "consts": cst, "w_in": np.ascontiguousarray(f("w_in")[0]), "wab": wab,
        "w_out": np.ascontiguousarray(f("w_out")[0]),
        "wr": np.ascontiguousarray(np.concatenate([f("w_router_group")[0], f("w_router_expert")[0]], axis=1)),
        "wg": np.ascontiguousarray(f("w_exp_gate")[0]), "wu": np.ascontiguousarray(f("w_exp_up")[0]),
        "wd": np.ascontiguousarray(f("w_exp_down")[0]),
        "w_pg": np.ascontiguousarray(f("w_ple_gate")[0]), "w_ple": np.ascontiguousarray(f("w_ple")[0]),
    }
    return shared


def kernel(**inputs):
    shared = _host_layout(inputs)
    x = np.asarray(inputs["x"], dtype=np.float32)
    p = np.asarray(inputs["p"], dtype=np.float32)[0]
    nc = build()
    in_maps = []
    for c in range(NCORES):
        m = dict(shared)
        m["x"] = np.ascontiguousarray(x[c])
        m["p"] = np.ascontiguousarray(p[c])
        in_maps.append(m)
    res = run_bass_kernel_spmd(nc, in_maps, core_ids=list(range(NCORES)))
    out = np.stack([np.asarray(r["out"], dtype=np.float32) for r in res.results], axis=0)
    return out.astype(inputs["x"].dtype, copy=False)
```
